# Optimizing a Trainium2 kernel written in Bass

```python
import math
import jax
import jax.numpy as jnp
from jax import lax
import numpy as np

D_MODEL = 2048
BATCH = 8
SEQ = 2048
DEPTH = 2

MIX_WIDTH = D_MODEL // 2
N_BRANCH = 3
HEAD_DIM = 128
FOX_HEADS = MIX_WIDTH // HEAD_DIM
RET_HEADS = MIX_WIDTH // HEAD_DIM
SSM_GROUP = 16
SSM_GROUPS = MIX_WIDTH // SSM_GROUP
SSM_STATE = 64
D_FF = ((8 * D_MODEL // 3 + 127) // 128) * 128
N_MOD = 9
Q_BLOCK = 128
RET_CHUNK = 128
RET_DECAY_BASE = 5.0
ROPE_BASE = 10000.0
EPS = 1e-6
DT_MIN = 0.001
DT_MAX = 0.1
IN_SPLITS = (MIX_WIDTH, MIX_WIDTH, MIX_WIDTH, FOX_HEADS, MIX_WIDTH,
             MIX_WIDTH, MIX_WIDTH, MIX_WIDTH, MIX_WIDTH, N_BRANCH * D_MODEL)
IN_WIDTH = 8 * MIX_WIDTH + FOX_HEADS + N_BRANCH * D_MODEL

kernel_name = "fox_s5_retnet_macaron_adaln_hybrid"


def rmsnorm(x, w):
    xf = x.astype(jnp.float32)
    y = xf * lax.rsqrt(jnp.mean(xf * xf, axis=-1, keepdims=True) + EPS)
    return (y * w.astype(jnp.float32)).astype(x.dtype)


def modulate(h, shift, scale):
    return h * (1.0 + scale[:, None, :]) + shift[:, None, :]


def swiglu(h, w1, w3, w2):
    return (jax.nn.silu(h @ w1) * (h @ w3)) @ w2


def split_cols(z, sizes):
    idx = []
    acc = 0
    for s in sizes[:-1]:
        acc += s
        idx.append(acc)
    return jnp.split(z, idx, axis=-1)


def fox_attention(q, k, v, f_logit):
    bsz, s_len, _ = q.shape
    nb = s_len // Q_BLOCK

    def heads(t):
        return t.reshape(bsz, s_len, FOX_HEADS, HEAD_DIM).transpose(0, 2, 1, 3)

    qh = heads(q) * (HEAD_DIM ** -0.5)
    kh = heads(k)
    vh = heads(v)
    cum_logf = jnp.cumsum(jax.nn.log_sigmoid(f_logit.astype(jnp.float32)), axis=1).transpose(0, 2, 1)
    q_blocks = qh.reshape(bsz, FOX_HEADS, nb, Q_BLOCK, HEAD_DIM).transpose(2, 0, 1, 3, 4)
    f_blocks = cum_logf.reshape(bsz, FOX_HEADS, nb, Q_BLOCK).transpose(2, 0, 1, 3)
    key_pos = jnp.arange(s_len)

    def one_block(args):
        q_i, f_i, i = args
        logits = jnp.einsum('bhqd,bhkd->bhqk', q_i, kh).astype(jnp.float32)
        logits = logits + f_i[..., :, None] - cum_logf[..., None, :]
        q_pos = i * Q_BLOCK + jnp.arange(Q_BLOCK)
        causal = key_pos[None, :] <= q_pos[:, None]
        logits = jnp.where(causal, logits, -jnp.inf)
        probs = jax.nn.softmax(logits, axis=-1).astype(vh.dtype)
        return jnp.einsum('bhqk,bhkd->bhqd', probs, vh)

    out = lax.map(one_block, (q_blocks, f_blocks, jnp.arange(nb)))
    return out.transpose(1, 0, 3, 2, 4).reshape(bsz, s_len, MIX_WIDTH)


def s5_ssm(u, a_re, a_im, log_dt, b_re, b_im, c_re, c_im, d_skip):
    f32 = jnp.float32
    bsz, s_len, _ = u.shape
    a_re = a_re.astype(f32)
    a_im = a_im.astype(f32)
    u_g = u.astype(f32).reshape(bsz, s_len, SSM_GROUPS, SSM_GROUP).transpose(1, 0, 2, 3)
    dt = jnp.exp(log_dt.astype(f32))[:, None]
    mag = jnp.exp(a_re * dt)
    ab_re = mag * jnp.cos(a_im * dt)
    ab_im = mag * jnp.sin(a_im * dt)
    den = a_re * a_re + a_im * a_im
    num_re = ab_re - 1.0
    coef_re = (num_re * a_re + ab_im * a_im) / den
    coef_im = (ab_im * a_re - num_re * a_im) / den
    b_re = b_re.astype(f32)
    b_im = b_im.astype(f32)
    bb_re = coef_re[..., None] * b_re - coef_im[..., None] * b_im
    bb_im = coef_re[..., None] * b_im + coef_im[..., None] * b_re
    bu_re = jnp.einsum('sbgn,gpn->sbgp', u_g, bb_re)
    bu_im = jnp.einsum('sbgn,gpn->sbgp', u_g, bb_im)
    a_seq_re = jnp.broadcast_to(ab_re, (s_len,) + ab_re.shape)
    a_seq_im = jnp.broadcast_to(ab_im, (s_len,) + ab_im.shape)

    def combine(e1, e2):
        a1r, a1i, b1r, b1i = e1
        a2r, a2i, b2r, b2i = e2
        ar = a1r * a2r - a1i * a2i
        ai = a1r * a2i + a1i * a2r
        br = a2r[:, None] * b1r - a2i[:, None] * b1i + b2r
        bi = a2r[:, None] * b1i + a2i[:, None] * b1r + b2i
        return ar, ai, br, bi

    _, _, x_re, x_im = lax.associative_scan(combine, (a_seq_re, a_seq_im, bu_re, bu_im), axis=0)
    y = (jnp.einsum('sbgp,gnp->sbgn', x_re, c_re.astype(f32))
         - jnp.einsum('sbgp,gnp->sbgn', x_im, c_im.astype(f32)))
    y = y + d_skip.astype(f32).reshape(SSM_GROUPS, SSM_GROUP) * u_g
    return y.transpose(1, 0, 2, 3).reshape(bsz, s_len, MIX_WIDTH)


def retention(q, k, v):
    f32 = jnp.float32
    bsz, s_len, _ = q.shape
    nc = s_len // RET_CHUNK
    half = HEAD_DIM // 2
    pos = jnp.arange(s_len, dtype=f32)
    inv_freq = 1.0 / (ROPE_BASE ** jnp.linspace(0.0, 1.0, half, dtype=f32))
    ang = pos[:, None] * inv_freq[None, :]
    cos = jnp.cos(ang)[:, None, :]
    sin = jnp.sin(ang)[:, None, :]

    def heads(t):
        return t.astype(f32).reshape(bsz, s_len, RET_HEADS, HEAD_DIM)

    def rot(t):
        t1, t2 = t[..., :half], t[..., half:]
        return jnp.concatenate([t1 * cos - t2 * sin, t1 * sin + t2 * cos], axis=-1)

    def chunks(t):
        return t.reshape(bsz, nc, RET_CHUNK, RET_HEADS, HEAD_DIM).transpose(1, 0, 3, 2, 4)

    qh = rot(heads(q))
    kh = rot(heads(k)) * (HEAD_DIM ** -0.5)
    vh = heads(v)
    log_gamma = jnp.log(1.0 - jnp.exp2(-RET_DECAY_BASE - jnp.arange(RET_HEADS, dtype=f32)))
    idx = jnp.arange(RET_CHUNK, dtype=f32)
    rel = idx[:, None] - idx[None, :]
    intra = jnp.where(rel[None] >= 0, jnp.exp(jnp.maximum(rel, 0.0)[None] * log_gamma[:, None, None]), 0.0)
    q_decay = jnp.exp((idx + 1.0)[None, :] * log_gamma[:, None])
    k_decay = jnp.exp((RET_CHUNK - 1.0 - idx)[None, :] * log_gamma[:, None])
    chunk_decay = jnp.exp(RET_CHUNK * log_gamma)

    def step(state, qkv):
        q_c, k_c, v_c = qkv
        scores = jnp.einsum('bhnd,bhmd->bhnm', q_c, k_c) * intra
        o = (jnp.einsum('bhnm,bhme->bhne', scores, v_c)
             + jnp.einsum('bhnd,bhde->bhne', q_c * q_decay[..., None], state))
        state = chunk_decay[:, None, None] * state + jnp.einsum('bhmd,bhme->bhde', k_c * k_decay[..., None], v_c)
        return state, o

    state0 = jnp.zeros((bsz, RET_HEADS, HEAD_DIM, HEAD_DIM), f32)
    _, o = lax.scan(step, state0, (chunks(qh), chunks(kh), chunks(vh)))
    o = o.transpose(1, 0, 3, 2, 4)
    mu = jnp.mean(o, axis=-1, keepdims=True)
    var = jnp.mean(jnp.square(o - mu), axis=-1, keepdims=True)
    o = (o - mu) * lax.rsqrt(var + EPS)
    return o.reshape(bsz, s_len, MIX_WIDTH)


def token_mixing(h, w_in, f_bias, a_re, a_im, log_dt, b_re, b_im, c_re, c_im, d_skip,
                 glu_w, glu_b, gn_w, w_branch, b_gate, w_out):
    bsz, s_len, _ = h.shape
    z = h @ w_in
    qa, ka, va, fa, us, qr, kr, vr, gr, gl = split_cols(z, IN_SPLITS)
    y_fox = fox_attention(qa, ka, va, fa + f_bias)
    y_ssm = jax.nn.gelu(s5_ssm(us, a_re, a_im, log_dt, b_re, b_im, c_re, c_im, d_skip).astype(h.dtype))
    y_ssm = y_ssm * jax.nn.sigmoid(y_ssm @ glu_w + glu_b)
    y_ret = (retention(qr, kr, vr) * gn_w).astype(h.dtype) * jax.nn.silu(gr)
    gates = jax.nn.sigmoid(gl.reshape(bsz, s_len, N_BRANCH, D_MODEL) + b_gate)
    merged = (gates[:, :, 0] * (y_fox @ w_branch[0])
              + gates[:, :, 1] * (y_ssm @ w_branch[1])
              + gates[:, :, 2] * (y_ret @ w_branch[2]))
    return merged @ w_out


def setup_inputs(seed: int = 0) -> dict:
    key = jax.random.key(seed)
    ks = jax.random.split(key, 26)
    f32 = jnp.float32
    L, D, W, F = DEPTH, D_MODEL, MIX_WIDTH, D_FF
    G, P, N = SSM_GROUPS, SSM_STATE, SSM_GROUP

    def nrm(k, shape, scale):
        return jax.random.normal(k, shape, f32) * scale

    n_idx = jnp.arange(P, dtype=f32)
    return {
        "x": nrm(ks[0], (BATCH, SEQ, D), 1.0),
        "c": nrm(ks[1], (BATCH, D), 1.0),
        "ada_w": nrm(ks[2], (L, D, N_MOD * D), 0.5 * D ** -0.5),
        "ada_b": nrm(ks[3], (L, N_MOD * D), 0.02),
        "norm_w": 1.0 + nrm(ks[4], (L, 3, D), 0.02),
        "final_norm_w": 1.0 + nrm(ks[5], (D,), 0.02),
        "ffn_w1": nrm(ks[6], (L, 2, D, F), D ** -0.5),
        "ffn_w3": nrm(ks[7], (L, 2, D, F), D ** -0.5),
        "ffn_w2": nrm(ks[8], (L, 2, F, D), F ** -0.5),
        "w_in": nrm(ks[9], (L, D, IN_WIDTH), D ** -0.5),
        "fox_f_bias": 3.0 + nrm(ks[10], (L, FOX_HEADS), 0.5),
        "ssm_A_re": -0.5 * jnp.exp(nrm(ks[11], (L, G, P), 0.05)),
        "ssm_A_im": math.pi * n_idx + nrm(ks[12], (L, G, P), 0.05),
        "ssm_log_dt": jax.random.uniform(ks[13], (L, G), f32, math.log(DT_MIN), math.log(DT_MAX)),
        "ssm_B_re": nrm(ks[14], (L, G, P, N), (2 * N) ** -0.5),
        "ssm_B_im": nrm(ks[15], (L, G, P, N), (2 * N) ** -0.5),
        "ssm_C_re": nrm(ks[16], (L, G, N, P), P ** -0.5),
        "ssm_C_im": nrm(ks[17], (L, G, N, P), P ** -0.5),
        "ssm_D": nrm(ks[18], (L, W), 1.0),
        "glu_w": nrm(ks[19], (L, W, W), W ** -0.5),
        "glu_b": nrm(ks[20], (L, W), 0.02),
        "ret_gn_w": 1.0 + nrm(ks[21], (L, W), 0.02),
        "w_branch": nrm(ks[22], (L, N_BRANCH, W, D), W ** -0.5),
        "b_gate": nrm(ks[23], (L, N_BRANCH, D), 0.02),
        "w_out": nrm(ks[24], (L, D, D), D ** -0.5),
    }


def reference(x, c, ada_w, ada_b, norm_w, final_norm_w, ffn_w1, ffn_w3, ffn_w2, w_in, fox_f_bias,
              ssm_A_re, ssm_A_im, ssm_log_dt, ssm_B_re, ssm_B_im, ssm_C_re, ssm_C_im, ssm_D,
              glu_w, glu_b, ret_gn_w, w_branch, b_gate, w_out):
    c_act = jax.nn.silu(c)
    for l in range(DEPTH):
        mod = c_act @ ada_w[l] + ada_b[l]
        sh1, sc1, g1, sh2, sc2, g2, sh3, sc3, g3 = jnp.split(mod, N_MOD, axis=-1)
        h = modulate(rmsnorm(x, norm_w[l, 0]), sh1, sc1)
        x = x + 0.5 * g1[:, None, :] * swiglu(h, ffn_w1[l, 0], ffn_w3[l, 0], ffn_w2[l, 0])
        h = modulate(rmsnorm(x, norm_w[l, 1]), sh2, sc2)
        y = token_mixing(h, w_in[l], fox_f_bias[l], ssm_A_re[l], ssm_A_im[l], ssm_log_dt[l],
                         ssm_B_re[l], ssm_B_im[l], ssm_C_re[l], ssm_C_im[l], ssm_D[l],
                         glu_w[l], glu_b[l], ret_gn_w[l], w_branch[l], b_gate[l], w_out[l])
        x = x + g2[:, None, :] * y.astype(x.dtype)
        h = modulate(rmsnorm(x, norm_w[l, 2]), sh3, sc3)
        x = x + 0.5 * g3[:, None, :] * swiglu(h, ffn_w1[l, 1], ffn_w3[l, 1], ffn_w2[l, 1])
    return rmsnorm(x, final_norm_w)
```

```python
import math
import numpy as np
from contextlib import ExitStack
import concourse.bass as bass
import concourse.mybir as mybir

F32 = mybir.dt.float32
BF16 = mybir.dt.bfloat16
I32 = mybir.dt.int32
AF = mybir.ActivationFunctionType
ALU = mybir.AluOpType

SEM_M = 4096


class Reg:
    __slots__ = ("name", "lw", "rd")

    def __init__(self, name):
        self.name = name
        self.lw = None
        self.rd = []


class EngState:
    def __init__(self, fw, name, handle, dma_like=False, self_sync=True):
        self.fw = fw
        self.name = name
        self.h = handle
        self.count = 0
        self.sems = []
        self.clock = {}
        self.op_clocks = [None]
        self.dma_like = dma_like
        self.self_sync = self_sync

    def sem_for(self, seq):
        e = (seq - 1) // SEM_M
        while len(self.sems) <= e:
            self.sems.append(self.fw.new_sem(f"{self.name}_e{len(self.sems)}"))
        mult = 16 if self.dma_like else 1
        return self.sems[e], ((seq - 1) % SEM_M + 1) * mult


class FW:
    def __init__(self, nc):
        self.nc = nc
        self.es = ExitStack()
        self.root_es = self.es
        self.nsem = 0
        self.pe = EngState(self, "pe", nc.tensor, self_sync=False)
        self.act = EngState(self, "act", nc.scalar)
        self.dve = EngState(self, "dve", nc.vector)
        self.pool = EngState(self, "pool", nc.gpsimd)
        self.sp = EngState(self, "sp", nc.sync)
        self.engs = {e.name: e for e in (self.pe, self.act, self.dve, self.pool, self.sp)}
        self.ndma = 24
        self.dmas = [EngState(self, f"dma{i}", None, dma_like=True) for i in range(self.ndma)]
        for d in self.dmas:
            self.engs[d.name] = d
        self.dma_rr = 0
        self.nwaits = 0

    def new_sem(self, name):
        self.nsem += 1
        return self.root_es.enter_context(self.nc.semaphore(name))

    def sb(self, name, shape, dt):
        self.nalloc = getattr(self, "nalloc", 0) + 1
        return self.es.enter_context(self.nc.sbuf_tensor(f"{name}_{self.nalloc}", list(shape), dt))

    def ps(self, name, shape, dt=F32):
        self.nalloc = getattr(self, "nalloc", 0) + 1
        return self.es.enter_context(self.nc.psum_tensor(f"{name}_{self.nalloc}", list(shape), dt))

    def dram(self, name, shape, dt, kind):
        return self.nc.dram_tensor(name, list(shape), dt, kind=kind).ap()

    def _collect(self, reads, writes):
        deps = {}
        for r in reads:
            if r.lw is not None:
                e, s = r.lw
                if deps.get(e.name, (None, 0))[1] < s:
                    deps[e.name] = (e, s)
        for w in writes:
            if w.lw is not None:
                e, s = w.lw
                if deps.get(e.name, (None, 0))[1] < s:
                    deps[e.name] = (e, s)
            for (e, s) in w.rd:
                if deps.get(e.name, (None, 0))[1] < s:
                    deps[e.name] = (e, s)
        return deps

    def _wait_deps(self, E, deps):
        for name, (e, s) in deps.items():
            if e is E and not E.self_sync:
                continue
            if E.clock.get(name, 0) >= s:
                continue
            sem, val = e.sem_for(s)
            E.h.wait_ge(sem, val)
            self.nwaits += 1
            oc = e.op_clocks[s]
            for k, v in oc.items():
                if E.clock.get(k, 0) < v:
                    E.clock[k] = v
            if E.clock.get(name, 0) < s:
                E.clock[name] = s

    def _commit(self, E, seq, reads, writes):
        for w in writes:
            w.lw = (E, seq)
            w.rd = []
        for r in reads:
            r.rd.append((E, seq))

    def op(self, E, fn, reads=(), writes=()):
        self._wait_deps(E, self._collect(reads, writes))
        ins = fn()
        E.count += 1
        seq = E.count
        sem, val = E.sem_for(seq)
        ins.then_inc(sem, 1)
        E.clock[E.name] = seq if not E.self_sync else E.clock.get(E.name, 0)
        ck = dict(E.clock)
        ck[E.name] = seq
        E.op_clocks.append(ck)
        self._commit(E, seq, reads, writes)
        return ins

    def group(self, E, fns, reads=(), writes=()):
        self._wait_deps(E, self._collect(reads, writes))
        ins = None
        for fn in fns:
            ins = fn()
        E.count += 1
        seq = E.count
        sem, val = E.sem_for(seq)
        ins.then_inc(sem, 1)
        E.clock[E.name] = seq if not E.self_sync else E.clock.get(E.name, 0)
        ck = dict(E.clock)
        ck[E.name] = seq
        E.op_clocks.append(ck)
        self._commit(E, seq, reads, writes)
        return ins

    def dma(self, Q, out, in_, reads=(), writes=(), **kw):
        d = self.dmas[self.dma_rr]
        self.dma_rr = (self.dma_rr + 1) % self.ndma
        deps = self._collect(reads, writes)
        if d.count > 0:
            if deps.get(d.name, (None, 0))[1] < d.count:
                deps[d.name] = (d, d.count)
        self._wait_deps(Q, deps)
        ins = Q.h.dma_start(out=out, in_=in_, **kw)
        d.count += 1
        seq = d.count
        sem, val = d.sem_for(seq)
        ins.then_inc(sem, 16)
        ck = dict(Q.clock)
        ck[d.name] = seq
        d.op_clocks.append(ck)
        self._commit(d, seq, reads, writes)
        return ins

    def wait_all(self, E, regs):
        deps = self._collect(regs, ())
        self._wait_deps(E, deps)

    def barrier(self):
        for E in (self.pe, self.act, self.dve, self.pool, self.sp):
            deps = {}
            for e in self.engs.values():
                if e.count > 0:
                    deps[e.name] = (e, e.count)
            ss = E.self_sync
            E.self_sync = True
            self._wait_deps(E, deps)
            E.self_sync = ss

    def scope(self):
        return Scope(self)

    def close(self):
        self.es.close()


class Scope:
    def __init__(self, fw):
        self.fw = fw

    def __enter__(self):
        self.saved = self.fw.es
        self.fw.es = ExitStack()
        return self

    def __exit__(self, *a):
        self.fw.barrier()
        self.fw.es.close()
        self.fw.es = self.saved
        return False


class Ring:
    def __init__(self, fw, name, shape, dt, n, psum=False):
        self.tiles = []
        self.regs = []
        for i in range(n):
            t = fw.ps(f"{name}{i}", shape, dt) if psum else fw.sb(f"{name}{i}", shape, dt)
            self.tiles.append(t)
            self.regs.append(Reg(f"{name}{i}"))
        self.i = 0
        self.n = n

    def next(self):
        t, r = self.tiles[self.i], self.regs[self.i]
        self.i = (self.i + 1) % self.n
        return t, r

from concourse.bass_utils import run_bass_kernel_spmd

D = 2048
S = 2048
L = 2
FF = 5504
NFC = FF // 128
W = 1024
KC = D // 128
TB = 512
NTB = S // TB
EPS = 1e-6
NMOD = 9
FPARTS = [(0, 11), (11, 11), (22, 11), (33, 10)]


class KB:
    def __init__(self, nc, stages):
        self.nc = nc
        self.fw = FW(nc)
        self.stages = stages
        fw = self.fw
        dr = fw.dram
        self.xT_in = dr("xT", [D, S], F32, "ExternalInput")
        self.cT = dr("cT", [128, KC], F32, "ExternalInput")
        self.ada_w = [dr(f"ada_w{l}", [D, NMOD * D], F32, "ExternalInput") for l in range(L)]
        self.ada_bT = [dr(f"ada_bT{l}", [128, NMOD * KC], F32, "ExternalInput") for l in range(L)]
        self.norm_wT = [dr(f"norm_wT{l}", [128, 3 * KC], F32, "ExternalInput") for l in range(L)]
        self.fnorm_wT = dr("fnorm_wT", [128, KC], F32, "ExternalInput")
        self.w1 = [[dr(f"w1_{l}_{i}", [D, FF], F32, "ExternalInput") for i in range(2)] for l in range(L)]
        self.w3 = [[dr(f"w3_{l}_{i}", [D, FF], F32, "ExternalInput") for i in range(2)] for l in range(L)]
        self.w2 = [[dr(f"w2_{l}_{i}", [FF, D], F32, "ExternalInput") for i in range(2)] for l in range(L)]
        self.outT = dr("outT", [D, S], F32, "ExternalOutput")
        self.xres = dr("xres", [D, S], F32, "Internal")
        self.x_regs = [[Reg(f"x_{dc}_{tb}") for tb in range(NTB)] for dc in range(KC)]
        self.x_cur = self.xT_in
        self.ones_bf = fw.sb("ones_bf", [128, 128], BF16)
        self.ones_r = Reg("ones_bf")
        self.eps_t = fw.sb("eps_t", [128, 1], F32)
        self.eps_r = Reg("eps")
        fw.op(fw.dve, lambda: nc.vector.memset(self.ones_bf[:], 1.0), writes=[self.ones_r])
        fw.op(fw.dve, lambda: nc.vector.memset(self.eps_t[:], EPS), writes=[self.eps_r])
        self.banks = [fw.ps(f"bank{i}", [128, 512], F32) for i in range(8)]
        self.bank_r = [Reg(f"bank{i}") for i in range(8)]
        self.mod = [fw.sb(f"mod{l}", [128, NMOD * KC], F32) for l in range(L)]
        self.mod_r = [Reg(f"mod{l}") for l in range(L)]
        self.avec = fw.sb("avec", [128, L * 3 * KC], F32)
        self.gvec = fw.sb("gvec", [128, L * 3 * KC], F32)
        self.av_r = Reg("avec")
        self.gv_r = Reg("gvec")
        self.fnw = fw.sb("fnw", [128, KC], F32)
        self.fnw_r = Reg("fnw")
        self.hT = fw.sb("hT", [128, KC, S], BF16)
        self.hT_r = [[Reg(f"hT_{c}_{tb}") for tb in range(NTB)] for c in range(KC)]

    def prologue(self):
        nc, fw = self.nc, self.fw
        self.cact = fw.sb("cact", [128, KC], F32)
        self.cact_r = Reg("cact")
        self.cact_bf = fw.sb("cact_bf", [128, KC], BF16)
        with self.fw.scope():
            c_sb = fw.sb("c_sb", [128, KC], F32)
            c_r = Reg("c_sb")
            fw.dma(fw.sp, c_sb[:], self.cT[:, :], writes=[c_r])
            fw.op(fw.act, lambda: nc.scalar.activation(out=self.cact[:], in_=c_sb[:], func=AF.Silu),
                  reads=[c_r], writes=[self.cact_r])
            fw.op(fw.act, lambda: nc.scalar.activation(out=self.cact_bf[:], in_=c_sb[:], func=AF.Silu),
                  reads=[c_r], writes=[self.cact_r])
            fw.dma(fw.sp, self.fnw[:], self.fnorm_wT[:, :], writes=[self.fnw_r])
            mg = self.mod_gen(0, 0)
            for _ in range(NMOD * D // 256):
                next(mg)
            self.prologue_extra()
            for _ in mg:
                pass

    def prologue_extra(self):
        pass

    def mod_gen(self, l, bank_i):
        nc, fw = self.nc, self.fw
        cact, cact_r = self.cact, self.cact_r
        NB = 256
        ring = Ring(fw, "adaw", [128, KC, NB], BF16, 2)
        cact = self.cact_bf
        adab = fw.sb("adab", [128, NMOD * KC], F32)
        adab_r = Reg("adab")
        nw = fw.sb("nw", [128, 3 * KC], F32)
        nw_r = Reg("nw")
        if True:
            bank, bank_r = self.banks[bank_i], self.bank_r[bank_i]
            aw = self.ada_w[l].rearrange("(kc p) n -> p kc n", p=128)
            for nb in range(NMOD * D // NB):
                t, r = ring.next()
                fw.dma(fw.pool, t[:], aw[:, :, nb * NB:(nb + 1) * NB], writes=[r])
                for j in range(NB // 128):
                    col = nb * (NB // 128) + j
                    fns = []
                    for kc in range(KC):
                        fns.append(lambda kc=kc, j=j, col=col, t=t: nc.tensor.matmul(
                            bank[:, col:col + 1], lhsT=t[:, kc, j * 128:(j + 1) * 128],
                            rhs=cact[:, kc:kc + 1], start=(kc == 0), stop=(kc == KC - 1)))
                    fw.group(fw.pe, fns, reads=[r, cact_r], writes=[bank_r])
                yield
            fw.dma(fw.sp, adab[:], self.ada_bT[l][:, :], writes=[adab_r])
            fw.dma(fw.sp, nw[:], self.norm_wT[l][:, :], writes=[nw_r])
            fw.op(fw.dve, lambda l=l: nc.vector.tensor_tensor(
                out=self.mod[l][:], in0=bank[:, 0:NMOD * KC], in1=adab[:], op=ALU.add),
                reads=[bank_r, adab_r], writes=[self.mod_r[l]])
            for s in range(3):
                sc = self.mod[l][:, (3 * s + 1) * KC:(3 * s + 2) * KC]
                gt = self.mod[l][:, (3 * s + 2) * KC:(3 * s + 3) * KC]
                o = (l * 3 + s) * KC
                fw.op(fw.dve, lambda sc=sc, s=s, o=o: nc.vector.scalar_tensor_tensor(
                    out=self.avec[:, o:o + KC], in0=sc, scalar=1.0, in1=nw[:, s * KC:(s + 1) * KC],
                    op0=ALU.add, op1=ALU.mult), reads=[self.mod_r[l], nw_r], writes=[self.av_r])
                gm = 1.0 if s == 1 else 0.5
                fw.op(fw.dve, lambda gt=gt, o=o, gm=gm: nc.vector.tensor_scalar(
                    out=self.gvec[:, o:o + KC], in0=gt, scalar1=gm, scalar2=None, op0=ALU.mult),
                    reads=[self.mod_r[l]], writes=[self.gv_r])

    def norm_phase(self, a_ap, b_ap, a_regs, out_kind="hT", out_dram=None):
        nc, fw = self.nc, self.fw
        NB = 256
        self.xblk_ring = Ring(fw, "xblk", [128, KC, NB], F32, 2)
        self.sq_ring = Ring(fw, "sq", [128, KC, NB], BF16, 2)
        self.rs_ring = Ring(fw, "rs", [128, NB], F32, 2)
        self.rstd_ring = Ring(fw, "rstd", [128, NB], F32, 2)
        self.tmp_ring = Ring(fw, "ntmp", [128, NB], F32, 3)
        xsrc = self.x_cur.rearrange("(c p) t -> p c t", p=128)
        for tb8 in range(S // NB):
            ts = slice(tb8 * NB, (tb8 + 1) * NB)
            tb = tb8 // 2
            xb, xb_r = self.xblk_ring.next()
            fw.dma(fw.sp, xb[:], xsrc[:, :, ts], reads=[self.x_regs[c][tb] for c in range(KC)], writes=[xb_r])
            sq, sq_r = self.sq_ring.next()
            fw.op(fw.act, lambda: nc.scalar.activation(out=sq[:], in_=xb[:], func=AF.Square),
                  reads=[xb_r], writes=[sq_r])
            bank, bank_r = self.banks[0], self.bank_r[0]
            fns = [lambda c=c: nc.tensor.matmul(bank[:, 0:NB], lhsT=self.ones_bf[:], rhs=sq[:, c, :],
                                                start=(c == 0), stop=(c == KC - 1)) for c in range(KC)]
            fw.group(fw.pe, fns, reads=[sq_r, self.ones_r], writes=[bank_r])
            rs, rs_r = self.rs_ring.next()
            fw.op(fw.act, lambda: nc.scalar.activation(out=rs[:], in_=bank[:, 0:NB], func=AF.Sqrt,
                                                       bias=self.eps_t[:], scale=1.0 / D),
                  reads=[bank_r, self.eps_r], writes=[rs_r])
            rstd, rstd_r = self.rstd_ring.next()
            fw.op(fw.dve, lambda: nc.vector.reciprocal(out=rstd[:], in_=rs[:]), reads=[rs_r], writes=[rstd_r])
            for c in range(KC):
                tmp, tmp_r = self.tmp_ring.next()
                fw.op(fw.dve, lambda c=c, tmp=tmp: nc.vector.scalar_tensor_tensor(
                    out=tmp[:], in0=xb[:, c, :], scalar=a_ap[:, c:c + 1], in1=rstd[:],
                    op0=ALU.mult, op1=ALU.mult), reads=[xb_r, rstd_r] + a_regs, writes=[tmp_r])
                if out_kind == "hT":
                    fw.op(fw.act, lambda c=c, tmp=tmp: nc.scalar.activation(
                        out=self.hT[:, c, ts], in_=tmp[:], func=AF.Identity, bias=b_ap[:, c:c + 1], scale=1.0),
                        reads=[tmp_r] + a_regs, writes=[self.hT_r[c][tb]])
                else:
                    fw.dma(fw.sp, out_dram[c * 128:(c + 1) * 128, ts], tmp[:], reads=[tmp_r],
                           writes=[self.out_regs[c][tb]])

    def ffn(self, l, i):
        with self.fw.scope():
            self._ffn(l, i)

    def _ffn(self, l, i):
        nc, fw = self.nc, self.fw
        s = 0 if i == 0 else 2
        o = (l * 3 + s) * KC
        a_ap = self.avec[:, o:o + KC]
        b_ap = self.mod[l][:, (3 * s) * KC:(3 * s + 1) * KC]
        g_ap = self.gvec[:, o:o + KC]
        with fw.scope():
            self.norm_phase(a_ap, b_ap, [self.av_r, self.mod_r[l]])
        if True:
            self.w1_ring = Ring(fw, "w1t", [128, KC, 256], BF16, 2)
            self.w3_ring = Ring(fw, "w3t", [128, KC, 256], BF16, 2)
            self.w2_ring = Ring(fw, "w2t", [128, 11, 512], BF16, 2)
            self.w13_ring = True
            self.GT = fw.sb("GT", [128, 11, S], BF16)
            self.GT_r = [[Reg(f"GT_{f}_{tb}") for tb in range(NTB)] for f in range(11)]
            self.sil_ring = Ring(fw, "sil", [128, TB], F32, 3)
            self.xo_ring = Ring(fw, "xo", [128, TB], F32, 6)
            self.abank_i = 0
            self.bbank_i = 0
        w1 = self.w1[l][i].rearrange("(kc p) f -> p kc f", p=128)
        w3 = self.w3[l][i].rearrange("(kc p) f -> p kc f", p=128)
        w2 = self.w2[l][i].rearrange("(fc p) d -> p fc d", p=128)
        for (c0, n) in FPARTS:
            blocks = []
            c = c0
            while c < c0 + n:
                nb = min(2, c0 + n - c)
                blocks.append((c, nb))
                c += nb
            for (bc, nb) in blocks:
                w1t, w1r = self.w1_ring.next()
                w3t, w3r = self.w3_ring.next()
                fs = slice(bc * 128, (bc + nb) * 128)
                fw.dma(fw.pool, w1t[:, :, 0:nb * 128], w1[:, :, fs], writes=[w1r])
                fw.dma(fw.pool, w3t[:, :, 0:nb * 128], w3[:, :, fs], writes=[w3r])
                for fi in range(nb):
                    fcl = bc + fi - c0
                    for tb in range(NTB):
                        ts = slice(tb * TB, (tb + 1) * TB)
                        ia = (self.abank_i % 2) * 2
                        self.abank_i += 1
                        ba, ba_r = self.banks[ia], self.bank_r[ia]
                        bb, bb_r = self.banks[ia + 1], self.bank_r[ia + 1]
                        hregs = [self.hT_r[kc][tb] for kc in range(KC)]
                        fns = [lambda kc=kc, w1t=w1t, fi=fi, ba=ba, ts=ts: nc.tensor.matmul(
                            ba[:], lhsT=w1t[:, kc, fi * 128:(fi + 1) * 128], rhs=self.hT[:, kc, ts],
                            start=(kc == 0), stop=(kc == KC - 1)) for kc in range(KC)]
                        fw.group(fw.pe, fns, reads=[w1r] + hregs, writes=[ba_r])
                        fns = [lambda kc=kc, w3t=w3t, fi=fi, bb=bb, ts=ts: nc.tensor.matmul(
                            bb[:], lhsT=w3t[:, kc, fi * 128:(fi + 1) * 128], rhs=self.hT[:, kc, ts],
                            start=(kc == 0), stop=(kc == KC - 1)) for kc in range(KC)]
                        fw.group(fw.pe, fns, reads=[w3r] + hregs, writes=[bb_r])
                        sil, sil_r = self.sil_ring.next()
                        fw.op(fw.act, lambda sil=sil, ba=ba: nc.scalar.activation(out=sil[:], in_=ba[:], func=AF.Silu),
                              reads=[ba_r], writes=[sil_r])
                        fw.op(fw.dve, lambda sil=sil, bb=bb, fcl=fcl, ts=ts: nc.vector.tensor_tensor(
                            out=self.GT[:, fcl, ts], in0=sil[:], in1=bb[:], op=ALU.mult),
                            reads=[sil_r, bb_r], writes=[self.GT_r[fcl][tb]])
            xsrc = self.x_cur
            for dq in range(4):
                w2t, w2r = self.w2_ring.next()
                fw.dma(fw.pool, w2t[:, 0:n, :], w2[:, c0:c0 + n, dq * 512:(dq + 1) * 512], writes=[w2r])
                for dci in range(4):
                    dc = dq * 4 + dci
                    for tb in range(NTB):
                        ts = slice(tb * TB, (tb + 1) * TB)
                        ib = 4 + (self.bbank_i % 4)
                        self.bbank_i += 1
                        bk, bk_r = self.banks[ib], self.bank_r[ib]
                        xo, xo_r = self.xo_ring.next()
                        fw.dma(fw.sp, xo[:], xsrc[dc * 128:(dc + 1) * 128, ts], reads=[self.x_regs[dc][tb]], writes=[xo_r])
                        fns = [lambda f=f, w2t=w2t, dci=dci, bk=bk, ts=ts: nc.tensor.matmul(
                            bk[:], lhsT=w2t[:, f, dci * 128:(dci + 1) * 128], rhs=self.GT[:, f, ts],
                            start=(f == 0), stop=(f == n - 1)) for f in range(n)]
                        fw.group(fw.pe, fns, reads=[w2r] + [self.GT_r[f][tb] for f in range(n)], writes=[bk_r])
                        fw.op(fw.dve, lambda bk=bk, xo=xo, dc=dc: nc.vector.scalar_tensor_tensor(
                            out=xo[:], in0=bk[:], scalar=g_ap[:, dc:dc + 1], in1=xo[:], op0=ALU.mult, op1=ALU.add),
                            reads=[bk_r, xo_r, self.gv_r], writes=[xo_r])
                        fw.dma(fw.act, self.xres[dc * 128:(dc + 1) * 128, ts], xo[:], reads=[xo_r],
                               writes=[self.x_regs[dc][tb]])
            self.x_cur = self.xres

    def final(self):
        with self.fw.scope():
            self._final()

    def _final(self):
        nc, fw = self.nc, self.fw
        self.out_regs = [[Reg(f"o_{c}_{tb}") for tb in range(NTB)] for c in range(KC)]
        self.norm_phase(self.fnw, None, [self.fnw_r], out_kind="out", out_dram=self.outT)
        allr = [r for rr in self.out_regs for r in rr]
        fw.wait_all(fw.sp, allr)

    def build(self):
        self.prologue()
        for st in self.stages:
            if st[0] == "ffn":
                self.ffn(st[1], st[2])
        self.final()
        self.fw.close()


WIN = 14344
SBK = [0, 1, 6, 7]
OFF = dict(qa=0, ka=1024, va=2048, fa=3072, us=3080, qr=4104, kr=5128, vr=6152, gr=7176, gl=8200)
HD = 128
NH = 8
QS = HD ** -0.5


class KBM(KB):
    def __init__(self, nc, stages, debug=False):
        super().__init__(nc, stages)
        fw = self.fw
        dr = fw.dram
        self.debug = debug
        self.w_in = [dr(f"w_in{l}", [D, WIN], F32, "ExternalInput") for l in range(L)]
        self.fbias = [dr(f"fbias{l}", [8, 1], F32, "ExternalInput") for l in range(L)]
        self.glu_w = [dr(f"glu_w{l}", [W, W], F32, "ExternalInput") for l in range(L)]
        self.glu_bT = [dr(f"glu_bT{l}", [128, 8], F32, "ExternalInput") for l in range(L)]
        self.gn_wT = [dr(f"gn_wT{l}", [128, 8], F32, "ExternalInput") for l in range(L)]
        self.w_br = [dr(f"w_br{l}", [3 * W, D], F32, "ExternalInput") for l in range(L)]
        self.b_gateT = [dr(f"b_gateT{l}", [128, 3 * KC], F32, "ExternalInput") for l in range(L)]
        self.w_out = [dr(f"w_out{l}", [D, D], F32, "ExternalInput") for l in range(L)]
        self.sA_reS = [dr(f"sA_reS{l}", [128, 32], F32, "ExternalInput") for l in range(L)]
        self.sA_imS = [dr(f"sA_imS{l}", [128, 32], F32, "ExternalInput") for l in range(L)]
        self.sldtS = [dr(f"sldtS{l}", [128, 32], F32, "ExternalInput") for l in range(L)]
        self.sA_reR = [dr(f"sA_reR{l}", [128, 512], F32, "ExternalInput") for l in range(L)]
        self.sA_imR = [dr(f"sA_imR{l}", [128, 512], F32, "ExternalInput") for l in range(L)]
        self.sldtR = [dr(f"sldtR{l}", [128, 512], F32, "ExternalInput") for l in range(L)]
        self.sB_reR = [dr(f"sB_reR{l}", [128, 512], F32, "ExternalInput") for l in range(L)]
        self.sB_imR = [dr(f"sB_imR{l}", [128, 512], F32, "ExternalInput") for l in range(L)]
        self.sC_reS = [dr(f"sC_reS{l}", [128, 512], F32, "ExternalInput") for l in range(L)]
        self.sC_imS = [dr(f"sC_imS{l}", [128, 512], F32, "ExternalInput") for l in range(L)]
        self.sD_T = [dr(f"sD_T{l}", [128, 8], F32, "ExternalInput") for l in range(L)]
        self.c_tri = dr("c_tri", [128, 128], F32, "ExternalInput")
        self.c_rotC = dr("c_rotC", [128, S], F32, "ExternalInput")
        self.c_rotS = dr("c_rotS", [128, S], F32, "ExternalInput")
        self.c_gfull = dr("c_gfull", [8, 128, 512], F32, "ExternalInput")
        self.c_ttri = dr("c_ttri", [8, 128, 512], F32, "ExternalInput")
        self.c_mrow = dr("c_mrow", [128, 8], F32, "ExternalInput")
        self.c_mst = dr("c_mst", [128, 2], F32, "ExternalInput")
        kind = "ExternalOutput" if debug else "Internal"
        self.yf_d = dr("yf_d", [W, S], BF16, kind)
        self.ys_d = dr("ys_d", [W, S], BF16, kind)
        self.yr_d = dr("yr_d", [W, S], BF16, kind)
        self.y_regs = {n: [[Reg(f"{n}_{c}_{tb}") for tb in range(NTB)] for c in range(8)] for n in ("yf", "ys", "yr")}
        self.sst_d = []
        self.sst_r = []
        for l in range(L):
            self.sst_d.append({"rS": dr(f"sst_rS{l}", [128, 32], F32, "Internal"),
                               "cosT": dr(f"sst_cos{l}", [128, 4096], F32, "Internal"),
                               "sinT": dr(f"sst_sin{l}", [128, 4096], F32, "Internal"),
                               "BD0": dr(f"sst_bd0{l}", [128, 4096], BF16, "Internal"),
                               "BD1": dr(f"sst_bd1{l}", [128, 4096], BF16, "Internal"),
                               "CBD0": dr(f"sst_cbd0{l}", [128, 1024], BF16, "Internal"),
                               "CBD1": dr(f"sst_cbd1{l}", [128, 1024], BF16, "Internal")})
            self.sst_r.append(Reg(f"sst{l}"))
        self.pd = {}
        self.pd_r = {}
        for n, shp, dt in (("qa", [W, S], BF16), ("ka", [W, S], BF16), ("va", [S, W], BF16), ("vr", [S, W], BF16),
                           ("qr", [W, S], F32), ("kr", [W, S], F32), ("gr", [W, S], F32), ("g", [W, S], BF16)):
            self.pd[n] = dr(n + "_d", shp, dt, "Internal")
            if n in ("va", "vr"):
                self.pd_r[n] = [Reg(f"{n}_d{t}") for t in range(16)]
            else:
                self.pd_r[n] = [[Reg(f"{n}_d{c}_{tb}") for tb in range(NTB)] for c in range(8)]
        self.ones_f = fw.sb("ones_f", [128, 1], F32)
        self.ones_fr = Reg("ones_f")
        fw.op(fw.dve, lambda: nc.vector.memset(self.ones_f[:], 1.0), writes=[self.ones_fr])

    def prologue_extra(self):
        self.sst_done = set()
        ls = sorted({st[1] for st in self.stages if st[0] == "mix" and (len(st) < 3 or "ssm" in st[2])})
        if ls and self.sst_in_prologue:
            self.ssm_precompute(ls[0])

    def win_cols(self, l, col0, ncols):
        return self.w_in[l].rearrange("(kc p) n -> p kc n", p=128)[:, :, col0:col0 + ncols]

    def projA(self, wsrc, ncols, K, rhs_fn, rhs_regs, consume, banks, ring, Mchunk=128):
        nc, fw = self.nc, self.fw
        c = 0
        ci = 0
        while c < ncols:
            nt = min(256, ncols - c)
            wt, wr = ring.next()
            fw.dma(fw.pool, wt[:, 0:K, 0:nt], wsrc[:, :, c:c + nt], writes=[wr])
            for j in range(0, nt, Mchunk):
                m = min(Mchunk, nt - j)
                for tb in range(NTB):
                    ts = slice(tb * TB, (tb + 1) * TB)
                    bi = banks[self._bk % len(banks)]
                    self._bk += 1
                    bk, bk_r = self.banks[bi], self.bank_r[bi]
                    fns = [lambda kc=kc: nc.tensor.matmul(bk[0:m, :], lhsT=wt[:, kc, j:j + m], rhs=rhs_fn(kc, ts),
                                                         start=(kc == 0), stop=(kc == K - 1)) for kc in range(K)]
                    fw.group(fw.pe, fns, reads=[wr] + rhs_regs(tb), writes=[bk_r])
                    consume(bk, bk_r, ci, tb, m)
                ci += 1
            c += nt

    def projA_gen(self, wsrc, ncols, K, rhs_fn, rhs_regs, consume, banks, ring):
        nc, fw = self.nc, self.fw
        c = 0
        ci = 0
        while c < ncols:
            nt = min(256, ncols - c)
            wt, wr = ring.next()
            fw.dma(fw.pool, wt[:, 0:K, 0:nt], wsrc[:, :, c:c + nt], writes=[wr])
            for j in range(0, nt, 128):
                m = min(128, nt - j)
                for tb in range(NTB):
                    ts = slice(tb * TB, (tb + 1) * TB)
                    bi = banks[self._bk % len(banks)]
                    self._bk += 1
                    bk, bk_r = self.banks[bi], self.bank_r[bi]
                    fns = [lambda kc=kc: nc.tensor.matmul(bk[0:m, :], lhsT=wt[:, kc, j:j + m], rhs=rhs_fn(kc, ts),
                                                         start=(kc == 0), stop=(kc == K - 1)) for kc in range(K)]
                    fw.group(fw.pe, fns, reads=[wr] + rhs_regs(tb), writes=[bk_r])
                    consume(bk, bk_r, ci, tb, m)
                    yield
                ci += 1
            c += nt

    def projB_gen(self, l, col0, ncols, dst, dst_r, banks, ring, st_ring):
        nc, fw = self.nc, self.fw
        wsrc = self.win_cols(l, col0, ncols)
        for c in range(0, ncols, 256):
            wt, wr = ring.next()
            fw.dma(fw.pool, wt[:, :, 0:256], wsrc[:, :, c:c + 256], writes=[wr])
            for tkb in range(16):
                tb = tkb // 4
                bi = banks[self._bk % len(banks)]
                self._bk += 1
                bk, bk_r = self.banks[bi], self.bank_r[bi]
                fns = [lambda kc=kc: nc.tensor.matmul(bk[:, 0:256], lhsT=self.hT[:, kc, tkb * 128:(tkb + 1) * 128],
                                                     rhs=wt[:, kc, 0:256], start=(kc == 0), stop=(kc == KC - 1))
                       for kc in range(KC)]
                fw.group(fw.pe, fns, reads=[wr] + self.h_regs(tb), writes=[bk_r])
                st, st_r = st_ring.next()
                fw.op(fw.act, lambda: nc.scalar.activation(out=st[:], in_=bk[:, 0:256], func=AF.Copy), reads=[bk_r], writes=[st_r])
                fw.dma(fw.sp, dst[tkb * 128:(tkb + 1) * 128, c:c + 256], st[:], reads=[st_r], writes=[dst_r[tkb]])
                yield

    def proj_items(self, l):
        nc, fw = self.nc, self.fw
        wring = Ring(fw, "wA", [128, KC, 256], BF16, 2)
        stb = Ring(fw, "pstb", [128, TB], BF16, 3)
        stf = Ring(fw, "pstf", [128, TB], F32, 2)
        stv = Ring(fw, "pstv", [128, 256], BF16, 2)

        def mk(name, func, scale, ring):
            def cons(bk, bk_r, ci, tb, m):
                st, st_r = ring.next()
                fw.op(fw.act, lambda: nc.scalar.activation(out=st[:], in_=bk[:], func=func, scale=scale), reads=[bk_r], writes=[st_r])
                fw.dma(fw.sp, self.pd[name][ci * 128:(ci + 1) * 128, tb * TB:(tb + 1) * TB], st[:], reads=[st_r],
                       writes=[self.pd_r[name][ci][tb]])
            return cons
        A = lambda name, func, scale, ring: self.projA_gen(self.win_cols(l, OFF[name], W), W, KC, self.h_rhs, self.h_regs,
                                                            mk(name, func, scale, ring), [6, 7], wring)
        yield from A("qa", AF.Copy, QS, stb)
        yield from A("ka", AF.Copy, 1.0, stb)
        yield from self.projB_gen(l, OFF["va"], W, self.pd["va"], self.pd_r["va"], [6, 7], wring, stv)
        yield from A("qr", AF.Copy, 1.0, stf)
        yield from A("kr", AF.Copy, 1.0, stf)
        yield from self.projB_gen(l, OFF["vr"], W, self.pd["vr"], self.pd_r["vr"], [6, 7], wring, stv)
        yield from A("gr", AF.Silu, 1.0, stf)

    def h_rhs(self, kc, ts):
        return self.hT[:, kc, ts]

    def h_regs(self, tb):
        return [self.hT_r[kc][tb] for kc in range(KC)]

    def projB(self, l, col0, ncols, vtm, vtm_r, banks, ring):
        nc, fw = self.nc, self.fw
        wsrc = self.win_cols(l, col0, ncols)
        for c in range(0, ncols, 256):
            wt, wr = ring.next()
            fw.dma(fw.pool, wt[:, :, 0:256], wsrc[:, :, c:c + 256], writes=[wr])
            for tkb in range(16):
                tb = tkb // 4
                bi = banks[self._bk % len(banks)]
                self._bk += 1
                bk, bk_r = self.banks[bi], self.bank_r[bi]
                fns = [lambda kc=kc: nc.tensor.matmul(bk[:, 0:256], lhsT=self.hT[:, kc, tkb * 128:(tkb + 1) * 128],
                                                     rhs=wt[:, kc, 0:256], start=(kc == 0), stop=(kc == KC - 1))
                       for kc in range(KC)]
                fw.group(fw.pe, fns, reads=[wr] + self.h_regs(tb), writes=[bk_r])
                fw.op(fw.act, lambda: nc.scalar.activation(out=vtm[:, tkb, c:c + 256], in_=bk[:, 0:256], func=AF.Copy),
                      reads=[bk_r], writes=[vtm_r[tkb]])

    def mixer(self, l):
        nc, fw = self.nc, self.fw
        s = 1
        o = (l * 3 + s) * KC
        a_ap = self.avec[:, o:o + KC]
        b_ap = self.mod[l][:, (3 * s) * KC:(3 * s + 1) * KC]
        self._bk = 0
        with fw.scope():
            self.norm_phase(a_ap, b_ap, [self.av_r, self.mod_r[l]])
        if "ssm" in self.parts:
            with fw.scope():
                self.ssm(l)
        if "fox" in self.parts:
            with fw.scope():
                self.fox(l)
        if "ret" in self.parts:
            with fw.scope():
                self.ret(l)
        if "merge" in self.parts:
            with fw.scope():
                self.merge(l)

    def fox(self, l):
        nc, fw = self.nc, self.fw
        vtm = fw.sb("vtm", [128, 16, W], BF16)
        vtm_r = [Reg(f"vtm{t}") for t in range(16)]
        kaug = [fw.sb(f"kaug{t}", [128, S], BF16) for t in range(2)]
        qaug = [fw.sb(f"qaug{t}", [128, S], BF16) for t in range(2)]
        kaug_r = [Reg(f"kaug{t}") for t in range(2)]
        qaug_r = [Reg(f"qaug{t}") for t in range(2)]
        tri = fw.sb("tri", [128, 128], BF16)
        tri_r = Reg("tri")
        with fw.scope():
            trif = fw.sb("trif", [128, 128], F32)
            trif_r = Reg("trif")
            fw.dma(fw.sp, trif[:], self.c_tri[:, :], writes=[trif_r])
            fw.op(fw.dve, lambda: nc.vector.tensor_copy(out=tri[:], in_=trif[:]), reads=[trif_r], writes=[tri_r])
            for t in range(2):
                fw.op(fw.dve, lambda t=t: nc.vector.memset(kaug[t][:], 1.0), writes=[kaug_r[t]])
                fw.op(fw.dve, lambda t=t: nc.vector.memset(qaug[t][:], 1.0), writes=[qaug_r[t]])
            wfa = fw.sb("wfa", [128, KC, 8], BF16)
            wfa_r = Reg("wfa")
            fw.dma(fw.pool, wfa[:], self.win_cols(l, OFF["fa"], 8), writes=[wfa_r])
            fb = fw.sb("fb", [8, 1], F32)
            fb_r = Reg("fb")
            nfb = fw.sb("nfb", [8, 1], F32)
            nfb_r = Reg("nfb")
            fw.dma(fw.sp, fb[:], self.fbias[l][:, :], writes=[fb_r])
            fw.op(fw.dve, lambda: nc.vector.tensor_scalar(out=nfb[:], in0=fb[:], scalar1=-1.0, scalar2=None, op0=ALU.mult),
                  reads=[fb_r], writes=[nfb_r])
            e_t = fw.sb("e_t", [8, S], F32)
            e_r = Reg("e_t")
            for tb in range(NTB):
                ts = slice(tb * TB, (tb + 1) * TB)
                bk, bk_r = self.banks[6 + tb % 2], self.bank_r[6 + tb % 2]
                fns = [lambda kc=kc: nc.tensor.matmul(bk[0:8, :], lhsT=wfa[:, kc, 0:8], rhs=self.hT[:, kc, ts],
                                                     start=(kc == 0), stop=(kc == KC - 1)) for kc in range(KC)]
                fw.group(fw.pe, fns, reads=[wfa_r] + self.h_regs(tb), writes=[bk_r])
                fw.op(fw.act, lambda: nc.scalar.activation(out=e_t[:, ts], in_=bk[0:8, :], func=AF.Exp,
                                                           bias=nfb[:], scale=-1.0),
                      reads=[bk_r, nfb_r], writes=[e_r])
            lf = fw.sb("lf", [8, S], F32)
            lf_r = Reg("lf")
            fw.op(fw.act, lambda: nc.scalar.activation(out=lf[:], in_=e_t[:], func=AF.Ln, bias=self.ones_f[0:8, :], scale=1.0),
                  reads=[e_r, self.ones_fr], writes=[lf_r])
            G = fw.sb("G", [8, S], F32)
            G_r = Reg("G")
            fw.op(fw.dve, lambda: nc.vector.tensor_tensor_scan(
                out=G[:], data0=self.ones_f[0:8, 0:1].to_broadcast([8, S]), data1=lf[:], initial=0.0,
                op0=ALU.mult, op1=ALU.add), reads=[lf_r, self.ones_fr], writes=[G_r])
            Gs = [fw.sb(f"G{i}", [8, S], BF16) for i in range(3)]
            NGs = [fw.sb(f"NG{i}", [8, S], BF16) for i in range(3)]
            Gs_r = [Reg(f"G{i}") for i in range(3)]
            NGs_r = [Reg(f"NG{i}") for i in range(3)]
            R = fw.sb("Rres", [8, S], F32)
            R_r = Reg("Rres")
            cur, cur_r = G, G_r
            for i in range(3):
                fw.op(fw.dve, lambda i=i, cur=cur: nc.vector.tensor_copy(out=Gs[i][:], in_=cur[:]), reads=[cur_r], writes=[Gs_r[i]])
                fw.op(fw.dve, lambda i=i: nc.vector.tensor_scalar(out=NGs[i][:], in0=Gs[i][:], scalar1=-1.0, scalar2=None, op0=ALU.mult),
                      reads=[Gs_r[i]], writes=[NGs_r[i]])
                if i < 2:
                    fw.op(fw.dve, lambda i=i, cur=cur: nc.vector.tensor_tensor(out=R[:], in0=cur[:], in1=Gs[i][:], op=ALU.subtract),
                          reads=[cur_r, Gs_r[i]], writes=[R_r])
                    cur, cur_r = R, R_r
            for h in range(NH):
                t, j = h // 4, h % 4
                for i in range(3):
                    fw.dma(fw.sp, kaug[t][32 * j + 3 + i:32 * j + 4 + i, :], Gs[i][h:h + 1, :], reads=[Gs_r[i]], writes=[kaug_r[t]])
                    fw.dma(fw.sp, qaug[t][32 * j + i:32 * j + i + 1, :], NGs[i][h:h + 1, :], reads=[NGs_r[i]], writes=[qaug_r[t]])
        vsrc = self.pd["va"].rearrange("(t p) c -> p t c", p=128)
        for tkb in range(16):
            fw.dma(fw.sp, vtm[:, tkb, :], vsrc[:, tkb, :], reads=[self.pd_r["va"][tkb]], writes=[vtm_r[tkb]])
        qk_ring = Ring(fw, "qkT", [128, 2, 2, S], BF16, 2)
        pt_ring = Ring(fw, "PT", [128, 512], BF16, 4)
        rl_ring = Ring(fw, "rl", [128, 512], F32, 2)
        yo_ring = Ring(fw, "yo", [128, 512], BF16, 2)
        cnt = 0
        mg = self.mod_gen(l + 1, 5) if (l + 1 < L and self.defer_mod) else None
        for hp in range(4):
            qkt, qk_r = qk_ring.next()
            qT = qkt[:, 0]
            kT = qkt[:, 1]
            qT_r = [[qk_r] * NTB for hh in range(2)]
            kT_r = [[qk_r] * NTB for hh in range(2)]
            for hh in range(2):
                hq = hp * 2 + hh
                fw.dma(fw.sp, qkt[:, 0, hh, :], self.pd["qa"][hq * 128:(hq + 1) * 128, :], reads=self.pd_r["qa"][hq], writes=[qk_r])
                fw.dma(fw.sp, qkt[:, 1, hh, :], self.pd["ka"][hq * 128:(hq + 1) * 128, :], reads=self.pd_r["ka"][hq], writes=[qk_r])
            for hh in range(2):
                h = hp * 2 + hh
                t, j = h // 4, h % 4
                tp = (32 * j, 0)
                for qb in range(4):
                    Ob, Ob_r = self.banks[2 + cnt % 2], self.bank_r[2 + cnt % 2]
                    Lb, Lb_r = self.banks[4], self.bank_r[4]
                    cnt += 1
                    nkb = 4 * qb + 4
                    for _ in range(3):
                        if mg is not None:
                            try:
                                next(mg)
                            except StopIteration:
                                mg = None
                    def emit_qk(kb):
                        i = kb - 4 * qb
                        c0 = 128 * i if i >= 0 else 0
                        Sb, Sb_r = self.banks[SBK[kb % 4]], self.bank_r[SBK[kb % 4]]
                        qs = slice(qb * 512 + c0, (qb + 1) * 512)
                        ks = slice(kb * 128, (kb + 1) * 128)
                        fns = [lambda: nc.tensor.matmul(Sb[:, c0:512], lhsT=kT[:, hh, ks], rhs=qT[:, hh, qs], start=True, stop=False),
                               lambda: nc.tensor.matmul(Sb[:, c0:512], lhsT=kaug[t][32 * j:32 * j + 6, ks], rhs=qaug[t][32 * j:32 * j + 6, qs],
                                                        start=False, stop=True, tile_position=tp)]
                        fw.group(fw.pe, fns, reads=[kT_r[hh][kb // 4], qT_r[hh][qb], kaug_r[t], qaug_r[t]], writes=[Sb_r])
                    emit_qk(0)
                    emit_qk(1)
                    for kb in range(nkb):
                        i = kb - 4 * qb
                        c0 = 128 * i if i >= 0 else 0
                        Sb, Sb_r = self.banks[SBK[kb % 4]], self.bank_r[SBK[kb % 4]]
                        if kb + 2 < nkb:
                            emit_qk(kb + 2)
                        pt, pt_r = pt_ring.next()
                        fw.op(fw.act, lambda: nc.scalar.activation(out=pt[:, c0:512], in_=Sb[:, c0:512], func=AF.Exp),
                              reads=[Sb_r], writes=[pt_r])
                        if i >= 0:
                            fw.op(fw.dve, lambda: nc.vector.tensor_tensor(out=pt[:, c0:c0 + 128], in0=pt[:, c0:c0 + 128], in1=tri[:], op=ALU.mult),
                                  reads=[pt_r, tri_r], writes=[pt_r])
                        fns = [lambda: nc.tensor.matmul(Ob[:, c0:512], lhsT=vtm[:, kb, h * 128:(h + 1) * 128], rhs=pt[:, c0:512],
                                                        start=(kb == 0), stop=(kb == nkb - 1)),
                               lambda: nc.tensor.matmul(Lb[:, c0:512], lhsT=self.ones_bf[:], rhs=pt[:, c0:512],
                                                        start=(kb == 0), stop=(kb == nkb - 1))]
                        fw.group(fw.pe, fns, reads=[vtm_r[kb], pt_r, self.ones_r], writes=[Ob_r, Lb_r])
                    rl, rl_r = rl_ring.next()
                    fw.op(fw.dve, lambda: nc.vector.reciprocal(out=rl[:], in_=Lb[:]), reads=[Lb_r], writes=[rl_r])
                    yo, yo_r = yo_ring.next()
                    fw.op(fw.dve, lambda: nc.vector.tensor_tensor(out=yo[:], in0=Ob[:], in1=rl[:], op=ALU.mult),
                          reads=[Ob_r, rl_r], writes=[yo_r])
                    fw.dma(fw.sp, self.yf_d[h * 128:(h + 1) * 128, qb * 512:(qb + 1) * 512], yo[:], reads=[yo_r],
                           writes=[self.y_regs["yf"][h][qb]])
        if mg is not None:
            for _ in mg:
                pass

    def ret(self, l):
        nc, fw = self.nc, self.fw
        vtm = fw.sb("vtm", [128, 16, W], BF16)
        vtm_r = [Reg(f"vtm{t}") for t in range(16)]
        rotC = fw.sb("rotC", [128, S], F32)
        rotS = fw.sb("rotS", [128, S], F32)
        rot_r = Reg("rot")
        gnw = fw.sb("gnw", [128, 8], F32)
        gnw_r = Reg("gnw")
        fw.dma(fw.sp, rotC[:], self.c_rotC[:, :], writes=[rot_r])
        fw.dma(fw.sp, rotS[:], self.c_rotS[:, :], writes=[rot_r])
        fw.dma(fw.sp, gnw[:], self.gn_wT[l][:, :], writes=[gnw_r])
        vsrc = self.pd["vr"].rearrange("(t p) c -> p t c", p=128)
        for tkb in range(16):
            fw.dma(fw.sp, vtm[:, tkb, :], vsrc[:, tkb, :], reads=[self.pd_r["vr"][tkb]], writes=[vtm_r[tkb]])
        raw_ring = {n: Ring(fw, f"{n}raw", [128, S], F32, 2) for n in "qk"}
        sg_ring2 = Ring(fw, "sg", [128, S], F32, 2)
        sw = {n: fw.sb(f"{n}sw", [128, S], F32) for n in "qk"}
        sw_r = {n: Reg(f"{n}sw") for n in "qk"}
        rT = {n: fw.sb(f"{n}T", [128, S], BF16) for n in "qk"}
        rT_r = {n: Reg(f"{n}T") for n in "qk"}
        tab_ring = Ring(fw, "rtab", [128, 2, 512], F32, 1)
        at_ring = Ring(fw, "AT", [128, 512], BF16, 4)
        osb_ring = Ring(fw, "osb", [128, 512], F32, 1)
        obf_ring = Ring(fw, "obf", [128, 512], BF16, 2)
        sqb_ring = Ring(fw, "sqb", [128, 512], BF16, 2)
        m_ring = Ring(fw, "gm", [128, 512], F32, 1)
        v_ring = Ring(fw, "gv", [128, 512], F32, 1)
        yo_ring = Ring(fw, "yo", [128, 512], BF16, 2)
        cnt = 0
        pend = [None]

        def step_pend():
            if pend[0] is not None:
                try:
                    next(pend[0])
                except StopIteration:
                    pend[0] = None
        for h in range(NH):
            gamma = 1.0 - 2.0 ** (-5.0 - h)
            tab, tab_r = tab_ring.next()
            fw.dma(fw.sp, tab[:, 0, :], self.c_gfull[h], writes=[tab_r])
            fw.dma(fw.sp, tab[:, 1, :], self.c_ttri[h], writes=[tab_r])
            raw = {}
            raw_r = {}
            for n in "qk":
                raw[n], rr = raw_ring[n].next()
                raw_r[n] = [rr]
                fw.dma(fw.sp, raw[n][:], self.pd[n + "r"][h * 128:(h + 1) * 128, :], reads=self.pd_r[n + "r"][h], writes=[rr])
            sg, sgr = sg_ring2.next()
            sg_r = [sgr] * NTB
            fw.dma(fw.sp, sg[:], self.pd["gr"][h * 128:(h + 1) * 128, :], reads=self.pd_r["gr"][h], writes=[sgr])
            for n in "qk":
                fw.dma(fw.sp, sw[n][0:64, :], raw[n][64:128, :], reads=raw_r[n], writes=[sw_r[n]])
                fw.dma(fw.sp, sw[n][64:128, :], raw[n][0:64, :], reads=raw_r[n], writes=[sw_r[n]])
                RE, RH = (fw.pool, nc.gpsimd) if self.rot_on_pool else (fw.dve, nc.vector)
                fw.op(RE, lambda: RH.tensor_tensor(out=raw[n][:], in0=raw[n][:], in1=rotC[:], op=ALU.mult),
                      reads=raw_r[n] + [rot_r, sw_r[n]], writes=raw_r[n])
                fw.op(RE, lambda: RH.tensor_tensor(out=sw[n][:], in0=sw[n][:], in1=rotS[:], op=ALU.mult),
                      reads=[sw_r[n], rot_r], writes=[sw_r[n]])
                fw.op(RE, lambda: RH.tensor_tensor(out=rT[n][:], in0=raw[n][:], in1=sw[n][:], op=ALU.add),
                      reads=raw_r[n] + [sw_r[n]], writes=[rT_r[n]])

            for qb in range(4):
                Ob, Ob_r = self.banks[2 + cnt % 2], self.bank_r[2 + cnt % 2]
                Mb, Mb_r = self.banks[4], self.bank_r[4]
                Qb, Qb_r = self.banks[5], self.bank_r[5]
                cnt += 1
                nkb = 4 * qb + 4
                def emit_qk(kb):
                    i = kb - 4 * qb
                    c0 = 128 * i if i >= 0 else 0
                    Sb, Sb_r = self.banks[SBK[kb % 4]], self.bank_r[SBK[kb % 4]]
                    qs = slice(qb * 512 + c0, (qb + 1) * 512)
                    ks = slice(kb * 128, (kb + 1) * 128)
                    fw.group(fw.pe, [lambda: nc.tensor.matmul(Sb[:, c0:512], lhsT=rT["k"][:, ks], rhs=rT["q"][:, qs], start=True, stop=True)],
                             reads=[rT_r["k"], rT_r["q"]], writes=[Sb_r])
                emit_qk(0)
                emit_qk(1)
                for kb in range(nkb):
                    i = kb - 4 * qb
                    c0 = 128 * i if i >= 0 else 0
                    Sb, Sb_r = self.banks[SBK[kb % 4]], self.bank_r[SBK[kb % 4]]
                    if kb + 2 < nkb:
                        emit_qk(kb + 2)
                    at, at_r = at_ring.next()
                    if i >= 0:
                        cc = QS
                        tb_ap = tab[:, 1, 0:512 - c0]
                    else:
                        cc = QS * gamma ** (qb * 512 - kb * 128 - 128)
                        tb_ap = tab[:, 0, :]
                    fw.op(fw.dve, lambda: nc.vector.scalar_tensor_tensor(out=at[:, c0:512], in0=Sb[:, c0:512], scalar=float(cc), in1=tb_ap,
                                                                       op0=ALU.mult, op1=ALU.mult),
                          reads=[Sb_r, tab_r], writes=[at_r])
                    fw.group(fw.pe, [lambda: nc.tensor.matmul(Ob[:, c0:512], lhsT=vtm[:, kb, h * 128:(h + 1) * 128], rhs=at[:, c0:512],
                                                              start=(kb == 0), stop=(kb == nkb - 1))],
                             reads=[vtm_r[kb], at_r], writes=[Ob_r])
                    step_pend()
                def gn_gen(h=h, qb=qb, Ob=Ob, Ob_r=Ob_r, sg=sg, sg_r=sg_r):
                    osb, osb_r = osb_ring.next()
                    obf, obf_r = obf_ring.next()
                    sqb, sqb_r = sqb_ring.next()
                    fw.op(fw.act, lambda: nc.scalar.activation(out=osb[:], in_=Ob[:], func=AF.Copy), reads=[Ob_r], writes=[osb_r])
                    fw.op(fw.act, lambda: nc.scalar.activation(out=obf[:], in_=Ob[:], func=AF.Copy), reads=[Ob_r], writes=[obf_r])
                    fw.op(fw.act, lambda: nc.scalar.activation(out=sqb[:], in_=Ob[:], func=AF.Square), reads=[Ob_r], writes=[sqb_r])
                    fw.group(fw.pe, [lambda: nc.tensor.matmul(Mb[:], lhsT=self.ones_bf[:], rhs=obf[:], start=True, stop=True)],
                             reads=[obf_r, self.ones_r], writes=[Mb_r])
                    fw.group(fw.pe, [lambda: nc.tensor.matmul(Qb[:], lhsT=self.ones_bf[:], rhs=sqb[:], start=True, stop=True)],
                             reads=[sqb_r, self.ones_r], writes=[Qb_r])
                    yield
                    gm, gm_r = m_ring.next()
                    gv, gv_r = v_ring.next()
                    fw.op(fw.dve, lambda: nc.vector.tensor_scalar(out=gm[:], in0=Mb[:], scalar1=1.0 / HD, scalar2=None, op0=ALU.mult),
                          reads=[Mb_r], writes=[gm_r])
                    yield
                    fw.op(fw.dve, lambda: nc.vector.tensor_tensor(out=gv[:], in0=gm[:], in1=gm[:], op=ALU.mult), reads=[gm_r], writes=[gv_r])
                    yield
                    fw.op(fw.dve, lambda: nc.vector.scalar_tensor_tensor(out=gv[:], in0=Qb[:], scalar=1.0 / HD, in1=gv[:], op0=ALU.mult, op1=ALU.subtract),
                          reads=[Qb_r, gv_r], writes=[gv_r])
                    fw.op(fw.act, lambda: nc.scalar.activation(out=gv[:], in_=gv[:], func=AF.Sqrt, bias=self.eps_t[:], scale=1.0),
                          reads=[gv_r, self.eps_r], writes=[gv_r])
                    yield
                    fw.op(fw.dve, lambda: nc.vector.tensor_tensor(out=osb[:], in0=osb[:], in1=gm[:], op=ALU.subtract), reads=[osb_r, gm_r], writes=[osb_r])
                    yield
                    fw.op(fw.dve, lambda: nc.vector.reciprocal(out=gv[:], in_=gv[:]), reads=[gv_r], writes=[gv_r])
                    yield
                    fw.op(fw.dve, lambda: nc.vector.tensor_tensor(out=osb[:], in0=osb[:], in1=gv[:], op=ALU.mult), reads=[osb_r, gv_r], writes=[osb_r])
                    yield
                    yo, yo_r = yo_ring.next()
                    fw.op(fw.dve, lambda: nc.vector.scalar_tensor_tensor(out=yo[:], in0=osb[:], scalar=gnw[:, h:h + 1], in1=sg[:, qb * 512:(qb + 1) * 512],
                                                                       op0=ALU.mult, op1=ALU.mult),
                          reads=[osb_r, gnw_r, sg_r[qb]], writes=[yo_r])
                    fw.dma(fw.sp, self.yr_d[h * 128:(h + 1) * 128, qb * 512:(qb + 1) * 512], yo[:], reads=[yo_r],
                           writes=[self.y_regs["yr"][h][qb]])
                if self.gn_lazy:
                    if pend[0] is not None:
                        for _ in pend[0]:
                            pass
                    pend[0] = gn_gen()
                else:
                    for _ in gn_gen():
                        pass
        if pend[0] is not None:
            for _ in pend[0]:
                pass

    def merge(self, l):
        nc, fw = self.nc, self.fw
        o = (l * 3 + 1) * KC
        g_ap = self.gvec[:, o:o + KC]
        TP = 2 * TB
        wg_ring = Ring(fw, "wg", [128, KC, 128], BF16, 4)
        wb_ring = Ring(fw, "wb", [128, 8, 128], BF16, 4)
        bg = fw.sb("bg", [128, 3 * KC], F32)
        bg_r = Reg("bg")
        fw.dma(fw.sp, bg[:], self.b_gateT[l][:, :], writes=[bg_r])
        yb = [fw.sb(f"yb{br}", [128, 8, TP], BF16) for br in range(3)]
        yb_r = [Reg(f"yb{br}") for br in range(3)]
        mT = fw.sb("mT", [128, KC, TP], BF16)
        mT_r = [[Reg(f"mT{dc}_{t2}") for t2 in range(2)] for dc in range(KC)]
        sg_ring = Ring(fw, "msg", [128, TB], F32, 4)
        acc_ring = Ring(fw, "macc", [128, TB], F32, 4)
        t_ring = Ring(fw, "mt", [128, TB], F32, 2)
        xo_ring = Ring(fw, "xo", [128, TB], F32, 4)
        ysrc = [d.rearrange("(c p) t -> p c t", p=128) for d in (self.yf_d, self.ys_d, self.yr_d)]
        ynames = ["yf", "ys", "yr"]
        wbr = self.w_br[l].rearrange("(c p) d -> p c d", p=128)
        wo = self.w_out[l].rearrange("(kc p) d -> p kc d", p=128)
        gcnt = 0
        bcnt = 0
        ocnt = 0
        for tp in range(S // TP):
            for br in range(3):
                fw.dma(fw.sp, yb[br][:], ysrc[br][:, :, tp * TP:(tp + 1) * TP],
                       reads=[self.y_regs[ynames[br]][c][2 * tp + t2] for c in range(8) for t2 in range(2)], writes=[yb_r[br]])
            for dc in range(KC):
                accs = [acc_ring.next() for _ in range(2)]
                for br in range(3):
                    wg, wg_r = wg_ring.next()
                    fw.dma(fw.pool, wg[:], self.win_cols(l, OFF["gl"] + br * D + dc * 128, 128), writes=[wg_r])
                    wb, wb_r = wb_ring.next()
                    fw.dma(fw.pool, wb[:], wbr[:, br * 8:(br + 1) * 8, dc * 128:(dc + 1) * 128], writes=[wb_r])
                    for t2 in range(2):
                        tb = 2 * tp + t2
                        ts = slice(tb * TB, (tb + 1) * TB)
                        t2s = slice(t2 * TB, (t2 + 1) * TB)
                        gb, gb_r = self.banks[gcnt % 3], self.bank_r[gcnt % 3]
                        gcnt += 1
                        fns = [lambda kc=kc: nc.tensor.matmul(gb[:], lhsT=wg[:, kc, :], rhs=self.hT[:, kc, ts], start=(kc == 0), stop=(kc == KC - 1))
                               for kc in range(KC)]
                        fw.group(fw.pe, fns, reads=[wg_r] + self.h_regs(tb), writes=[gb_r])
                        sgt, sgt_r = sg_ring.next()
                        fw.op(fw.act, lambda: nc.scalar.activation(out=sgt[:], in_=gb[:], func=AF.Sigmoid, bias=bg[:, br * KC + dc:br * KC + dc + 1], scale=1.0),
                              reads=[gb_r, bg_r], writes=[sgt_r])
                        bb, bb_r = self.banks[3 + bcnt % 3], self.bank_r[3 + bcnt % 3]
                        bcnt += 1
                        fns = [lambda kc=kc: nc.tensor.matmul(bb[:], lhsT=wb[:, kc, :], rhs=yb[br][:, kc, t2s], start=(kc == 0), stop=(kc == 7))
                               for kc in range(8)]
                        fw.group(fw.pe, fns, reads=[wb_r, yb_r[br]], writes=[bb_r])
                        acc, acc_r = accs[t2]
                        if br == 0:
                            fw.op(fw.dve, lambda: nc.vector.tensor_tensor(out=acc[:], in0=bb[:], in1=sgt[:], op=ALU.mult),
                                  reads=[bb_r, sgt_r], writes=[acc_r])
                        else:
                            tt, tt_r = t_ring.next()
                            fw.op(fw.dve, lambda: nc.vector.tensor_tensor(out=tt[:], in0=bb[:], in1=sgt[:], op=ALU.mult),
                                  reads=[bb_r, sgt_r], writes=[tt_r])
                            if br == 1:
                                fw.op(fw.dve, lambda: nc.vector.tensor_tensor(out=acc[:], in0=acc[:], in1=tt[:], op=ALU.add),
                                      reads=[acc_r, tt_r], writes=[acc_r])
                            else:
                                fw.op(fw.dve, lambda: nc.vector.tensor_tensor(out=mT[:, dc, t2s], in0=acc[:], in1=tt[:], op=ALU.add),
                                      reads=[acc_r, tt_r], writes=[mT_r[dc][t2]])
            xsrc = self.x_cur
            for dc in range(KC):
                wg, wg_r = wg_ring.next()
                fw.dma(fw.pool, wg[:], wo[:, :, dc * 128:(dc + 1) * 128], writes=[wg_r])
                for t2 in range(2):
                    tb = 2 * tp + t2
                    ts = slice(tb * TB, (tb + 1) * TB)
                    t2s = slice(t2 * TB, (t2 + 1) * TB)
                    ob, ob_r = self.banks[6 + ocnt % 2], self.bank_r[6 + ocnt % 2]
                    ocnt += 1
                    xo, xo_r = xo_ring.next()
                    fw.dma(fw.act, xo[:], xsrc[dc * 128:(dc + 1) * 128, ts], reads=[self.x_regs[dc][tb]], writes=[xo_r])
                    fns = [lambda kc=kc: nc.tensor.matmul(ob[:], lhsT=wg[:, kc, :], rhs=mT[:, kc, t2s], start=(kc == 0), stop=(kc == KC - 1))
                           for kc in range(KC)]
                    fw.group(fw.pe, fns, reads=[wg_r] + [mT_r[kc][t2] for kc in range(KC)], writes=[ob_r])
                    fw.op(fw.dve, lambda: nc.vector.scalar_tensor_tensor(out=xo[:], in0=ob[:], scalar=g_ap[:, dc:dc + 1], in1=xo[:], op0=ALU.mult, op1=ALU.add),
                          reads=[ob_r, xo_r, self.gv_r], writes=[xo_r])
                    fw.dma(fw.sp, self.xres[dc * 128:(dc + 1) * 128, ts], xo[:], reads=[xo_r], writes=[self.x_regs[dc][tb]])
        self.x_cur = self.xres

    def cs16(self, th, N, c, s, tmp):
        nc, fw = self.nc, self.fw
        r = self._ssm_r
        t1, t2, t3 = tmp
        fw.op(fw.dve, lambda: nc.vector.tensor_scalar(out=t1[:], in0=th[:], scalar1=1.0 / 16, scalar2=None, op0=ALU.mult), reads=[r], writes=[r])
        fw.op(fw.act, lambda: nc.scalar.activation(out=s[:], in_=t1[:], func=AF.Sin), reads=[r], writes=[r])
        fw.op(fw.act, lambda: nc.scalar.activation(out=c[:], in_=t1[:], func=AF.Sin, bias=self.halfpi[:], scale=1.0), reads=[r], writes=[r])
        for _ in range(4):
            self.csq(c, s, tmp)

    def csq(self, c, s, tmp):
        nc, fw = self.nc, self.fw
        r = self._ssm_r
        t1, t2, t3 = tmp
        fw.op(fw.dve, lambda: nc.vector.tensor_tensor(out=t1[:], in0=c[:], in1=c[:], op=ALU.mult), reads=[r], writes=[r])
        fw.op(fw.dve, lambda: nc.vector.tensor_tensor(out=t2[:], in0=s[:], in1=s[:], op=ALU.mult), reads=[r], writes=[r])
        fw.op(fw.dve, lambda: nc.vector.tensor_tensor(out=t3[:], in0=c[:], in1=s[:], op=ALU.mult), reads=[r], writes=[r])
        fw.op(fw.dve, lambda: nc.vector.tensor_tensor(out=c[:], in0=t1[:], in1=t2[:], op=ALU.subtract), reads=[r], writes=[r])
        fw.op(fw.dve, lambda: nc.vector.tensor_scalar(out=s[:], in0=t3[:], scalar1=2.0, scalar2=None, op0=ALU.mult), reads=[r], writes=[r])

    def ssm_tiles(self):
        fw = self.fw
        rS = fw.sb("rS", [128, 32], F32)
        cosT = fw.sb("cosT", [128, 32, 128], F32)
        sinT = fw.sb("sinT", [128, 32, 128], F32)
        BD = [fw.sb(f"BD{i}", [128, 8, 4, 128], BF16) for i in range(2)]
        CBD = [fw.sb(f"CBD{i}", [128, 32, 32], BF16) for i in range(2)]
        return rS, cosT, sinT, BD, CBD

    def ssm_tab_pairs(self, l, tiles):
        rS, cosT, sinT, BD, CBD = tiles
        d = self.sst_d[l]
        return [(rS[:], d["rS"][:, :]), (cosT[:].rearrange("p j t -> p (j t)"), d["cosT"][:, :]),
                (sinT[:].rearrange("p j t -> p (j t)"), d["sinT"][:, :]),
                (BD[0][:].rearrange("p k j q -> p (k j q)"), d["BD0"][:, :]), (BD[1][:].rearrange("p k j q -> p (k j q)"), d["BD1"][:, :]),
                (CBD[0][:].rearrange("p j n -> p (j n)"), d["CBD0"][:, :]), (CBD[1][:].rearrange("p j n -> p (j n)"), d["CBD1"][:, :])]

    def ssm_precompute(self, l):
        fw = self.fw
        self._ssm_r = Reg(f"ssm_setup{l}")
        tiles = self.ssm_tiles()
        self.ssm_setup(l, *tiles)
        for sb_ap, d_ap in self.ssm_tab_pairs(l, tiles):
            fw.dma(fw.sp, d_ap, sb_ap, reads=[self._ssm_r], writes=[self.sst_r[l]])
        self.sst_done.add(l)

    def ssm(self, l):
        nc, fw = self.nc, self.fw
        V = nc.vector
        self._ssm_r = Reg("ssm_main")
        r = self._ssm_r
        tiles = self.ssm_tiles()
        rS, cosT, sinT, BD, CBD = tiles
        if l in self.sst_done:
            for sb_ap, d_ap in self.ssm_tab_pairs(l, tiles):
                fw.dma(fw.sp, sb_ap, d_ap, reads=[self.sst_r[l]], writes=[r])
        else:
            with fw.scope():
                self.ssm_setup(l, *tiles)
        DT = fw.sb("DT", [128, 8], F32)
        glub = fw.sb("glub", [128, 8], F32)
        fw.dma(fw.sp, DT[:], self.sD_T[l][:, :], writes=[r])
        fw.dma(fw.sp, glub[:], self.glu_bT[l][:, :], writes=[r])
        self.ssm_main(l, rS, cosT, sinT, BD, CBD, DT, glub, r)

    def ssm_setup(self, l, rS, cosT, sinT, BD, CBD):
        nc, fw = self.nc, self.fw
        V = nc.vector
        r = self._ssm_r
        self.halfpi = fw.sb("halfpi", [128, 1], F32)
        fw.op(fw.dve, lambda: V.memset(self.halfpi[:], math.pi / 2), writes=[r])

        def dve(fn):
            fw.op(fw.dve, fn, reads=[r], writes=[r])

        def act(fn):
            fw.op(fw.act, fn, reads=[r], writes=[r])

        if True:
            mrow = fw.sb("mrow", [128, 8], F32)
            mst = fw.sb("mst", [128, 2], F32)
            fw.dma(fw.sp, mrow[:], self.c_mrow[:, :], writes=[r])
            fw.dma(fw.sp, mst[:], self.c_mst[:, :], writes=[r])
            are = fw.sb("are", [128, 32], F32)
            aim = fw.sb("aim", [128, 32], F32)
            ldt = fw.sb("ldt", [128, 32], F32)
            fw.dma(fw.sp, are[:], self.sA_reS[l][:, :], writes=[r])
            fw.dma(fw.sp, aim[:], self.sA_imS[l][:, :], writes=[r])
            fw.dma(fw.sp, ldt[:], self.sldtS[l][:, :], writes=[r])
            dt = fw.sb("dt", [128, 32], F32)
            th = fw.sb("th", [128, 32], F32)
            c1 = fw.sb("c1", [128, 32], F32)
            s1 = fw.sb("s1", [128, 32], F32)
            tmpS = [fw.sb(f"tmpS{i}", [128, 32], F32) for i in range(3)]
            act(lambda: nc.scalar.activation(out=dt[:], in_=ldt[:], func=AF.Exp))
            dve(lambda: V.tensor_tensor(out=th[:], in0=are[:], in1=dt[:], op=ALU.mult))
            act(lambda: nc.scalar.activation(out=rS[:], in_=th[:], func=AF.Exp))
            dve(lambda: V.tensor_tensor(out=th[:], in0=aim[:], in1=dt[:], op=ALU.mult))
            self.cs16(th, 32, c1, s1, tmpS)
            tA = fw.sb("tA", [128, 32, 64], F32)
            tB = fw.sb("tB", [128, 32, 64], F32)
            dve(lambda: V.tensor_copy(out=cosT[:, :, 0:1], in_=c1[:, :].unsqueeze(2)))
            dve(lambda: V.tensor_copy(out=sinT[:, :, 0:1], in_=s1[:, :].unsqueeze(2)))
            n = 1
            while n < 128:
                pc = c1[:, :].unsqueeze(2).to_broadcast([128, 32, n])
                ps_ = s1[:, :].unsqueeze(2).to_broadcast([128, 32, n])
                dve(lambda: V.tensor_tensor(out=tA[:, :, 0:n], in0=cosT[:, :, 0:n], in1=pc, op=ALU.mult))
                dve(lambda: V.tensor_tensor(out=tB[:, :, 0:n], in0=sinT[:, :, 0:n], in1=ps_, op=ALU.mult))
                dve(lambda: V.tensor_tensor(out=cosT[:, :, n:2 * n], in0=tA[:, :, 0:n], in1=tB[:, :, 0:n], op=ALU.subtract))
                dve(lambda: V.tensor_tensor(out=tA[:, :, 0:n], in0=cosT[:, :, 0:n], in1=ps_, op=ALU.mult))
                dve(lambda: V.tensor_tensor(out=tB[:, :, 0:n], in0=sinT[:, :, 0:n], in1=pc, op=ALU.mult))
                dve(lambda: V.tensor_tensor(out=sinT[:, :, n:2 * n], in0=tA[:, :, 0:n], in1=tB[:, :, 0:n], op=ALU.add))
                n *= 2
                if n < 128:
                    self.csq(c1, s1, tmpS)
            Cre = fw.sb("Cre", [128, 32, 16], F32)
            Cim = fw.sb("Cim", [128, 32, 16], F32)
            fw.dma(fw.sp, Cre[:], self.sC_reS[l].rearrange("p (j n) -> p j n", n=16), writes=[r])
            fw.dma(fw.sp, Cim[:], self.sC_imS[l].rearrange("p (j n) -> p j n", n=16), writes=[r])
            for g2 in range(2):
                dve(lambda: V.tensor_scalar(out=CBD[0][:, :, g2 * 16:(g2 + 1) * 16], in0=Cre[:], scalar1=mst[:, g2:g2 + 1], scalar2=None, op0=ALU.mult))
                dve(lambda: V.tensor_scalar(out=CBD[1][:, :, g2 * 16:(g2 + 1) * 16], in0=Cim[:], scalar1=mst[:, g2:g2 + 1], scalar2=-1.0,
                                            op0=ALU.mult, op1=ALU.mult))
            R = {}
            for nm, src in (("are", self.sA_reR), ("aim", self.sA_imR), ("ldt", self.sldtR), ("Bre", self.sB_reR), ("Bim", self.sB_imR)):
                R[nm] = fw.sb("R" + nm, [128, 512], F32)
                fw.dma(fw.sp, R[nm][:], src[l][:, :], writes=[r])
            for nm in ("dt", "th", "mag", "c", "s", "abre", "abim", "den", "t1", "t2", "t3", "cre", "cim"):
                R[nm] = fw.sb("R" + nm, [128, 512], F32)
            act(lambda: nc.scalar.activation(out=R["dt"][:], in_=R["ldt"][:], func=AF.Exp))
            dve(lambda: V.tensor_tensor(out=R["th"][:], in0=R["are"][:], in1=R["dt"][:], op=ALU.mult))
            act(lambda: nc.scalar.activation(out=R["mag"][:], in_=R["th"][:], func=AF.Exp))
            dve(lambda: V.tensor_tensor(out=R["th"][:], in0=R["aim"][:], in1=R["dt"][:], op=ALU.mult))
            self.cs16(R["th"], 512, R["c"], R["s"], [R["t1"], R["t2"], R["t3"]])
            dve(lambda: V.tensor_tensor(out=R["abre"][:], in0=R["mag"][:], in1=R["c"][:], op=ALU.mult))
            dve(lambda: V.tensor_tensor(out=R["abim"][:], in0=R["mag"][:], in1=R["s"][:], op=ALU.mult))
            dve(lambda: V.tensor_scalar(out=R["abre"][:], in0=R["abre"][:], scalar1=-1.0, scalar2=None, op0=ALU.add))
            dve(lambda: V.tensor_tensor(out=R["t1"][:], in0=R["are"][:], in1=R["are"][:], op=ALU.mult))
            dve(lambda: V.tensor_tensor(out=R["t2"][:], in0=R["aim"][:], in1=R["aim"][:], op=ALU.mult))
            dve(lambda: V.tensor_tensor(out=R["den"][:], in0=R["t1"][:], in1=R["t2"][:], op=ALU.add))
            dve(lambda: V.reciprocal(out=R["den"][:], in_=R["den"][:]))
            dve(lambda: V.tensor_tensor(out=R["t1"][:], in0=R["abre"][:], in1=R["are"][:], op=ALU.mult))
            dve(lambda: V.tensor_tensor(out=R["t2"][:], in0=R["abim"][:], in1=R["aim"][:], op=ALU.mult))
            dve(lambda: V.tensor_tensor(out=R["t1"][:], in0=R["t1"][:], in1=R["t2"][:], op=ALU.add))
            dve(lambda: V.tensor_tensor(out=R["cre"][:], in0=R["t1"][:], in1=R["den"][:], op=ALU.mult))
            dve(lambda: V.tensor_tensor(out=R["t1"][:], in0=R["abim"][:], in1=R["are"][:], op=ALU.mult))
            dve(lambda: V.tensor_tensor(out=R["t2"][:], in0=R["abre"][:], in1=R["aim"][:], op=ALU.mult))
            dve(lambda: V.tensor_tensor(out=R["t1"][:], in0=R["t1"][:], in1=R["t2"][:], op=ALU.subtract))
            dve(lambda: V.tensor_tensor(out=R["cim"][:], in0=R["t1"][:], in1=R["den"][:], op=ALU.mult))
            dve(lambda: V.tensor_tensor(out=R["t1"][:], in0=R["cre"][:], in1=R["Bre"][:], op=ALU.mult))
            dve(lambda: V.tensor_tensor(out=R["t2"][:], in0=R["cim"][:], in1=R["Bim"][:], op=ALU.mult))
            dve(lambda: V.tensor_tensor(out=R["t3"][:], in0=R["t1"][:], in1=R["t2"][:], op=ALU.subtract))
            dve(lambda: V.tensor_tensor(out=R["t1"][:], in0=R["cre"][:], in1=R["Bim"][:], op=ALU.mult))
            dve(lambda: V.tensor_tensor(out=R["t2"][:], in0=R["cim"][:], in1=R["Bre"][:], op=ALU.mult))
            dve(lambda: V.tensor_tensor(out=R["t1"][:], in0=R["t1"][:], in1=R["t2"][:], op=ALU.add))
            for jj in range(4):
                for g2 in range(2):
                    mc = mrow[:, jj * 2 + g2:jj * 2 + g2 + 1]
                    dve(lambda: V.tensor_scalar(out=BD[0][:, :, jj, g2 * 64:(g2 + 1) * 64], in0=R["t3"][:].rearrange("p (k q) -> p k q", q=64),
                                                scalar1=mc, scalar2=None, op0=ALU.mult))
                    dve(lambda: V.tensor_scalar(out=BD[1][:, :, jj, g2 * 64:(g2 + 1) * 64], in0=R["t1"][:].rearrange("p (k q) -> p k q", q=64),
                                                scalar1=mc, scalar2=None, op0=ALU.mult))

    def ssm_main(self, l, rS, cosT, sinT, BD, CBD, DT, glub, r):
        nc, fw = self.nc, self.fw
        V = nc.vector
        usT = fw.sb("usT", [128, 8, S], BF16)
        usT_r = [[Reg(f"usT{c}_{tb}") for tb in range(NTB)] for c in range(8)]

        def cons_us(bk, bk_r, ci, tb, m):
            fw.op(fw.act, lambda: nc.scalar.activation(out=usT[:, ci, tb * TB:(tb + 1) * TB], in_=bk[:], func=AF.Copy),
                  reads=[bk_r], writes=[usT_r[ci][tb]])
        with fw.scope():
            wring = Ring(fw, "wA", [128, KC, 256], BF16, 2)
            self.projA(self.win_cols(l, OFF["us"], W), W, KC, self.h_rhs, self.h_regs, cons_us, [6, 7], wring)
        with fw.scope():
            self.ssm_scan(l, usT, usT_r, rS, cosT, sinT, BD, CBD, DT, r)
        with fw.scope():
            self.ssm_glu(l, glub, r)

    def ssm_scan(self, l, usT, usT_r, rS, cosT, sinT, BD, CBD, DT, r):
        nc, fw = self.nc, self.fw
        V = nc.vector
        pg = self.proj_items(l)
        go_ring = Ring(fw, "gout", [128, 512], BF16, 2)
        tmp_ring = Ring(fw, "st", [128, 512], F32, 4)
        b_ring = Ring(fw, "sb", [128, 2, 512], F32, 1)
        b_regs = (Reg("b_re"), Reg("b_im"))
        xb_ring = Ring(fw, "sxb", [128, 2, 512], BF16, 2)
        carry = [fw.sb(f"carry{i}", [128, 2, 4], F32) for i in range(2)]
        carry_r = [Reg(f"carry{i}") for i in range(2)]
        yp_ring = Ring(fw, "yp", [128, 512], F32, 1)
        g1_ring = Ring(fw, "g1", [128, 512], F32, 1)
        g2_ring = Ring(fw, "g2", [128, 512], F32, 1)
        units = [(kc, ch) for kc in range(8) for ch in range(16)]
        NU = len(units)
        st = {}

        def banksA(u):
            return (self.banks[0 + u % 2], self.bank_r[0 + u % 2], self.banks[2 + u % 2], self.bank_r[2 + u % 2])

        def emit_A(u):
            kc, ch = units[u]
            tb = ch // 4
            cs_ = slice(ch * 128, (ch + 1) * 128)
            Are, Are_r, Aim, Aim_r = banksA(u)
            for (A, A_r, bd) in ((Are, Are_r, BD[0]), (Aim, Aim_r, BD[1])):
                fns = [lambda jj=jj: nc.tensor.matmul(A[:, jj * 128:(jj + 1) * 128], lhsT=bd[:, kc, jj, :],
                                                     rhs=usT[:, kc, cs_], start=True, stop=True) for jj in range(4)]
                fw.group(fw.pe, fns, reads=[r, usT_r[kc][tb]], writes=[A_r])

        def emit_D(u):
            kc, ch = units[u]
            cosv = cosT[:, 4 * kc:4 * kc + 4, :]
            sinv = sinT[:, 4 * kc:4 * kc + 4, :]
            if ch == 0:
                fw.op(fw.dve, lambda: V.memset(carry[0][:], 0.0), writes=[carry_r[0]])
            Are, Are_r, Aim, Aim_r = banksA(u)
            Ar3 = Are[:].rearrange("p (j t) -> p j t", t=128)
            Ai3 = Aim[:].rearrange("p (j t) -> p j t", t=128)
            tt_ = [tmp_ring.next() for _ in range(4)]
            T3 = [t[:].rearrange("p (j t) -> p j t", t=128) for (t, _) in tt_]
            TR = [tr for (_, tr) in tt_]
            bt, _unused = b_ring.next()
            bt_r = b_regs
            b3 = [bt[:, i, :].rearrange("p (j t) -> p j t", t=128) for i in range(2)]
            bre_r, bim_r = bt_r
            fw.op(fw.dve, lambda: V.tensor_tensor(out=T3[0], in0=Ar3, in1=cosv, op=ALU.mult), reads=[Are_r, r], writes=[TR[0]])
            fw.op(fw.dve, lambda: V.tensor_tensor(out=T3[1], in0=Ai3, in1=sinv, op=ALU.mult), reads=[Aim_r, r], writes=[TR[1]])
            fw.op(fw.dve, lambda: V.tensor_tensor(out=T3[2], in0=Ai3, in1=cosv, op=ALU.mult), reads=[Aim_r, r], writes=[TR[2]])
            fw.op(fw.dve, lambda: V.tensor_tensor(out=T3[3], in0=Ar3, in1=sinv, op=ALU.mult), reads=[Are_r, r], writes=[TR[3]])
            fw.op(fw.dve, lambda: V.tensor_tensor(out=b3[0], in0=T3[0], in1=T3[1], op=ALU.add), reads=[TR[0], TR[1]], writes=[bre_r])
            fw.op(fw.dve, lambda: V.tensor_tensor(out=b3[1], in0=T3[2], in1=T3[3], op=ALU.subtract), reads=[TR[2], TR[3]], writes=[bim_r])
            cin, cin_r = carry[ch % 2], carry_r[ch % 2]
            cout, cout_r = carry[(ch + 1) % 2], carry_r[(ch + 1) % 2]
            xbanks = ((Are, Are_r), (Aim, Aim_r))
            for i in range(2):
                xo_, xo_r_ = xbanks[i]
                for jj in range(4):
                    j = 4 * kc + jj
                    fw.op(fw.dve, lambda: V.tensor_tensor_scan(
                        out=xo_[:, jj * 128:(jj + 1) * 128], data0=rS[:, j:j + 1].to_broadcast([128, 128]),
                        data1=bt[:, i, jj * 128:(jj + 1) * 128], initial=cin[:, i, jj:jj + 1], op0=ALU.mult, op1=ALU.add),
                        reads=[bt_r[i], cin_r, r], writes=[xo_r_])
            x3 = [Ar3, Ai3]
            xb, xb_r = xb_ring.next()
            xb3 = [xb[:, i, :].rearrange("p (j t) -> p j t", t=128) for i in range(2)]
            fw.op(fw.dve, lambda: V.tensor_tensor(out=T3[0], in0=x3[0], in1=cosv, op=ALU.mult), reads=[Are_r, r], writes=[TR[0]])
            fw.op(fw.dve, lambda: V.tensor_tensor(out=T3[2], in0=x3[0], in1=sinv, op=ALU.mult), reads=[Are_r, r], writes=[TR[2]])
            fw.op(fw.dve, lambda: V.tensor_tensor(out=T3[1], in0=x3[1], in1=sinv, op=ALU.mult), reads=[Aim_r, r], writes=[TR[1]])
            fw.op(fw.dve, lambda: V.tensor_tensor(out=T3[3], in0=x3[1], in1=cosv, op=ALU.mult), reads=[Aim_r, r], writes=[TR[3]])
            fw.op(fw.dve, lambda: V.tensor_tensor(out=xb3[0], in0=T3[0], in1=T3[1], op=ALU.subtract), reads=[TR[0], TR[1]], writes=[xb_r])
            fw.op(fw.dve, lambda: V.tensor_tensor(out=xb3[1], in0=T3[2], in1=T3[3], op=ALU.add), reads=[TR[2], TR[3]], writes=[xb_r])
            fw.op(fw.dve, lambda: V.tensor_tensor(out=cout[:, 0, :], in0=T3[0][:, :, 127], in1=T3[1][:, :, 127], op=ALU.subtract),
                  reads=[TR[0], TR[1]], writes=[cout_r])
            fw.op(fw.dve, lambda: V.tensor_tensor(out=cout[:, 1, :], in0=T3[2][:, :, 127], in1=T3[3][:, :, 127], op=ALU.add),
                  reads=[TR[2], TR[3]], writes=[cout_r])
            st[u] = (xb, xb_r)

        def emit_C(u):
            kc, ch = units[u]
            tb = ch // 4
            xb, xb_r = st.pop(u)
            Yb, Yb_r = self.banks[4 + tb % 2], self.bank_r[4 + tb % 2]
            yc = slice((ch % 4) * 128, (ch % 4 + 1) * 128)
            fns = []
            for jj in range(4):
                j = 4 * kc + jj
                fns.append(lambda jj=jj, j=j: nc.tensor.matmul(Yb[32 * jj:32 * jj + 32, yc], lhsT=CBD[0][:, j, :], rhs=xb[:, 0, jj * 128:(jj + 1) * 128],
                                                              start=True, stop=False, tile_position=(0, 32 * jj)))
                fns.append(lambda jj=jj, j=j: nc.tensor.matmul(Yb[32 * jj:32 * jj + 32, yc], lhsT=CBD[1][:, j, :], rhs=xb[:, 1, jj * 128:(jj + 1) * 128],
                                                              start=False, stop=True, tile_position=(0, 32 * jj)))
            fw.group(fw.pe, fns, reads=[xb_r, r], writes=[Yb_r])

        def emit_G(u):
            kc, ch = units[u]
            if ch % 4 != 3:
                return
            tb = ch // 4
            Yb, Yb_r = self.banks[4 + tb % 2], self.bank_r[4 + tb % 2]
            ts = slice(tb * TB, (tb + 1) * TB)
            yp, yp_r = yp_ring.next()
            fw.op(fw.dve, lambda: V.scalar_tensor_tensor(out=yp[:], in0=usT[:, kc, ts], scalar=DT[:, kc:kc + 1], in1=Yb[:], op0=ALU.mult, op1=ALU.add),
                  reads=[usT_r[kc][tb], Yb_r, r], writes=[yp_r])
            ga, ga_r = g1_ring.next()
            gb, gb_r = g2_ring.next()
            fw.op(fw.dve, lambda: V.tensor_tensor(out=ga[:], in0=yp[:], in1=yp[:], op=ALU.mult), reads=[yp_r], writes=[ga_r])
            fw.op(fw.dve, lambda: V.tensor_scalar(out=ga[:], in0=ga[:], scalar1=0.044715, scalar2=1.0, op0=ALU.mult, op1=ALU.add),
                  reads=[ga_r], writes=[ga_r])
            fw.op(fw.dve, lambda: V.tensor_tensor(out=ga[:], in0=ga[:], in1=yp[:], op=ALU.mult), reads=[ga_r, yp_r], writes=[ga_r])
            fw.op(fw.act, lambda: nc.scalar.activation(out=gb[:], in_=ga[:], func=AF.Sigmoid, scale=1.5957691216057308),
                  reads=[ga_r], writes=[gb_r])
            go, go_r = go_ring.next()
            fw.op(fw.dve, lambda: V.tensor_tensor(out=go[:], in0=yp[:], in1=gb[:], op=ALU.mult), reads=[yp_r, gb_r], writes=[go_r])
            fw.dma(fw.sp, self.pd["g"][kc * 128:(kc + 1) * 128, ts], go[:], reads=[go_r], writes=[self.pd_r["g"][kc][tb]])

        def step_pg(n):
            nonlocal pg
            for _ in range(n):
                if pg is not None:
                    try:
                        next(pg)
                    except StopIteration:
                        pg = None

        emit_A(0)
        for u in range(NU):
            emit_D(u)
            if u + 1 < NU:
                emit_A(u + 1)
            step_pg(3)
            emit_C(u)
            if u >= 1:
                emit_G(u - 1)
        emit_G(NU - 1)
        if pg is not None:
            for _ in pg:
                pass

    def ssm_glu(self, l, glub, r):
        nc, fw = self.nc, self.fw
        V = nc.vector
        gT = fw.sb("gT", [128, 8, S], BF16)
        gT_r = [[Reg(f"gT{c}_{tb}") for tb in range(NTB)] for c in range(8)]
        gsrc = self.pd["g"].rearrange("(c p) t -> p c t", p=128)
        for c in range(8):
            fw.dma(fw.sp, gT[:, c, :], gsrc[:, c, :], reads=self.pd_r["g"][c], writes=gT_r[c])
        wring = Ring(fw, "wA", [128, KC, 256], BF16, 2)
        sg_ring = Ring(fw, "gsg", [128, 512], F32, 2)
        yo_ring = Ring(fw, "yo", [128, 512], BF16, 2)

        def cons_glu(bk, bk_r, ci, tb, m):
            ts = slice(tb * TB, (tb + 1) * TB)
            sgt, sgt_r = sg_ring.next()
            fw.op(fw.act, lambda: nc.scalar.activation(out=sgt[:], in_=bk[:], func=AF.Sigmoid, bias=glub[:, ci:ci + 1], scale=1.0),
                  reads=[bk_r, r], writes=[sgt_r])
            yo, yo_r = yo_ring.next()
            fw.op(fw.dve, lambda: V.tensor_tensor(out=yo[:], in0=gT[:, ci, ts], in1=sgt[:], op=ALU.mult), reads=[gT_r[ci][tb], sgt_r], writes=[yo_r])
            fw.dma(fw.sp, self.ys_d[ci * 128:(ci + 1) * 128, ts], yo[:], reads=[yo_r], writes=[self.y_regs["ys"][ci][tb]])
        gw = self.glu_w[l].rearrange("(kc p) n -> p kc n", p=128)
        self.projA(gw, W, 8, lambda kc, ts: gT[:, kc, ts], lambda tb: [gT_r[kc][tb] for kc in range(8)], cons_glu, [6, 7], wring)

    rot_on_pool = False
    sst_in_prologue = False
    gn_lazy = True

    def build(self):
        self.defer_mod = any(st[0] == "mix" and st[1] == 0 and (len(st) < 3 or "fox" in st[2]) for st in self.stages)
        self.prologue()
        if not self.defer_mod:
            with self.fw.scope():
                for _ in self.mod_gen(1, 0):
                    pass
        for st in self.stages:
            if st[0] == "ffn":
                self.ffn(st[1], st[2])
            elif st[0] == "mix":
                self.parts = st[2] if len(st) > 2 else ("ssm", "fox", "ret", "merge")
                self.mixer(st[1])
        self.final()
        self.fw.close()

def prep_shared(inp):
    m = {}
    for l in range(L):
        m[f"ada_w{l}"] = np.ascontiguousarray(inp["ada_w"][l])
        m[f"ada_bT{l}"] = np.ascontiguousarray(inp["ada_b"][l].reshape(9*KC, 128).T)
        m[f"norm_wT{l}"] = np.ascontiguousarray(inp["norm_w"][l].reshape(3*KC, 128).T)
        for i in range(2):
            m[f"w1_{l}_{i}"] = np.ascontiguousarray(inp["ffn_w1"][l, i])
            m[f"w3_{l}_{i}"] = np.ascontiguousarray(inp["ffn_w3"][l, i])
            m[f"w2_{l}_{i}"] = np.ascontiguousarray(inp["ffn_w2"][l, i])
    m["fnorm_wT"] = np.ascontiguousarray(inp["final_norm_w"].reshape(KC, 128).T)
    return m
def prep_core(inp, b):
    return {"xT": np.ascontiguousarray(inp["x"][b].T), "cT": np.ascontiguousarray(inp["c"][b].reshape(KC,128).T)}

def prep_shared_mix(inp, m):
    f32 = np.float32
    for l in range(L):
        m[f"w_in{l}"] = np.ascontiguousarray(inp["w_in"][l])
        m[f"fbias{l}"] = np.ascontiguousarray(inp["fox_f_bias"][l].reshape(8, 1))
        m[f"glu_w{l}"] = np.ascontiguousarray(inp["glu_w"][l])
        m[f"glu_bT{l}"] = np.ascontiguousarray(inp["glu_b"][l].reshape(8, 128).T)
        m[f"gn_wT{l}"] = np.ascontiguousarray(inp["ret_gn_w"][l].reshape(8, 128).T)
        m[f"w_br{l}"] = np.ascontiguousarray(inp["w_branch"][l].reshape(3 * 1024, 2048))
        m[f"b_gateT{l}"] = np.ascontiguousarray(inp["b_gate"][l].reshape(48, 128).T)
        m[f"w_out{l}"] = np.ascontiguousarray(inp["w_out"][l])
        A_re, A_im, ldt = inp["ssm_A_re"][l], inp["ssm_A_im"][l], inp["ssm_log_dt"][l]
        def stl(A):
            return np.ascontiguousarray(A.reshape(32, 2, 64).transpose(1, 2, 0).reshape(128, 32))
        def rowl(A):
            a = A.reshape(8, 8, 1, 64)
            a = np.broadcast_to(a, (8, 8, 16, 64))
            return np.ascontiguousarray(a.transpose(1, 2, 0, 3).reshape(128, 512))
        m[f"sA_reS{l}"] = stl(A_re)
        m[f"sA_imS{l}"] = stl(A_im)
        m[f"sldtS{l}"] = stl(np.broadcast_to(ldt[:, None], (64, 64)))
        m[f"sA_reR{l}"] = rowl(A_re)
        m[f"sA_imR{l}"] = rowl(A_im)
        m[f"sldtR{l}"] = rowl(np.broadcast_to(ldt[:, None], (64, 64)))
        def browl(B):
            b = B.reshape(8, 8, 64, 16)
            return np.ascontiguousarray(b.transpose(1, 3, 0, 2).reshape(128, 512))
        m[f"sB_reR{l}"] = browl(inp["ssm_B_re"][l])
        m[f"sB_imR{l}"] = browl(inp["ssm_B_im"][l])
        def cstl(C):
            c = C.reshape(32, 2, 16, 64)
            return np.ascontiguousarray(c.transpose(1, 3, 0, 2).reshape(128, 512))
        m[f"sC_reS{l}"] = cstl(inp["ssm_C_re"][l])
        m[f"sC_imS{l}"] = cstl(inp["ssm_C_im"][l])
        m[f"sD_T{l}"] = np.ascontiguousarray(inp["ssm_D"][l].reshape(8, 128).T)
    k = np.arange(128)
    m["c_tri"] = (k[:, None] <= k[None, :]).astype(f32)
    pos = np.arange(S, dtype=f32)
    inv_freq = (1.0 / (f32(10000.0) ** np.linspace(0.0, 1.0, 64, dtype=f32))).astype(f32)
    ang = (pos[:, None] * inv_freq[None, :]).astype(f32)
    cos = np.cos(ang).astype(f32).T
    sin = np.sin(ang).astype(f32).T
    m["c_rotC"] = np.ascontiguousarray(np.concatenate([cos, cos], 0))
    m["c_rotS"] = np.ascontiguousarray(np.concatenate([-sin, sin], 0))
    gf = np.zeros((8, 128, 512), f32)
    tt = np.zeros((8, 128, 512), f32)
    mm = np.arange(128)[:, None].astype(np.float64)
    nn = np.arange(512)[None, :].astype(np.float64)
    for h in range(8):
        lg = np.log(np.float64(1.0 - 2.0 ** (-5.0 - h)))
        gf[h] = np.exp((nn - mm + 128) * lg)
        tt[h] = np.where(nn >= mm, np.exp(np.maximum(nn - mm, 0) * lg), 0.0)
    m["c_gfull"] = gf
    m["c_ttri"] = tt
    c = np.arange(128)
    m["c_mrow"] = np.stack([((c // 32 == jj) & ((c // 16) % 2 == g2)) for jj in range(4) for g2 in range(2)], 1).astype(f32)
    m["c_mst"] = np.stack([(c // 64 == g2) for g2 in range(2)], 1).astype(f32)
    return m


_CACHE = {}


def _stages():
    st = []
    for l in range(L):
        st += [("ffn", l, 0), ("mix", l), ("ffn", l, 1)]
    return st


def kernel(**inputs):
    inp = {k: np.asarray(v) for k, v in inputs.items()}
    if "nc" not in _CACHE:
        nc = bass.Bass("TRN2", target_bir_lowering=False)
        kbo = KBM(nc, _stages(), debug=False)
        kbo.build()
        names = set()
        for a in nc.allocations:
            if isinstance(a, mybir.MemoryLocationSet) and a.kind == "ExternalInput":
                names.add(a.memorylocations[0].name)
        _CACHE["nc"] = nc
        _CACHE["names"] = names
    nc, names = _CACHE["nc"], _CACHE["names"]
    shared = prep_shared_mix(inp, prep_shared(inp))
    in_maps = []
    for b in range(8):
        m = {**shared, **prep_core(inp, b)}
        in_maps.append({k: v for k, v in m.items() if k in names})
    res = run_bass_kernel_spmd(nc, in_maps, core_ids=list(range(8)))
    out = np.stack([np.ascontiguousarray(np.asarray(r["outT"]).T) for r in res.results], 0)
    return out.astype(np.float32)
```

```python
import math
import numpy as np
from contextlib import ExitStack
import concourse.bass as bass
import concourse.mybir as mybir

F32 = mybir.dt.float32
BF16 = mybir.dt.bfloat16
I32 = mybir.dt.int32
AF = mybir.ActivationFunctionType
ALU = mybir.AluOpType

SEM_M = 4096


class Reg:
    __slots__ = ("name", "lw", "rd")

    def __init__(self, name):
        self.name = name
        self.lw = None
        self.rd = []


class EngState:
    def __init__(self, fw, name, handle, dma_like=False, self_sync=True):
        self.fw = fw
        self.name = name
        self.h = handle
        self.count = 0
        self.sems = []
        self.clock = {}
        self.op_clocks = [None]
        self.dma_like = dma_like
        self.self_sync = self_sync

    def sem_for(self, seq):
        e = (seq - 1) // SEM_M
        while len(self.sems) <= e:
            self.sems.append(self.fw.new_sem(f"{self.name}_e{len(self.sems)}"))
        mult = 16 if self.dma_like else 1
        return self.sems[e], ((seq - 1) % SEM_M + 1) * mult


class FW:
    def __init__(self, nc):
        self.nc = nc
        self.es = ExitStack()
        self.root_es = self.es
        self.nsem = 0
        self.pe = EngState(self, "pe", nc.tensor, self_sync=False)
        self.act = EngState(self, "act", nc.scalar)
        self.dve = EngState(self, "dve", nc.vector)
        self.pool = EngState(self, "pool", nc.gpsimd)
        self.sp = EngState(self, "sp", nc.sync)
        self.engs = {e.name: e for e in (self.pe, self.act, self.dve, self.pool, self.sp)}
        self.ndma = 24
        self.dmas = [EngState(self, f"dma{i}", None, dma_like=True) for i in range(self.ndma)]
        for d in self.dmas:
            self.engs[d.name] = d
        self.dma_rr = 0
        self.nwaits = 0

    def new_sem(self, name):
        self.nsem += 1
        return self.root_es.enter_context(self.nc.semaphore(name))

    def sb(self, name, shape, dt):
        self.nalloc = getattr(self, "nalloc", 0) + 1
        return self.es.enter_context(self.nc.sbuf_tensor(f"{name}_{self.nalloc}", list(shape), dt))

    def ps(self, name, shape, dt=F32):
        self.nalloc = getattr(self, "nalloc", 0) + 1
        return self.es.enter_context(self.nc.psum_tensor(f"{name}_{self.nalloc}", list(shape), dt))

    def dram(self, name, shape, dt, kind):
        return self.nc.dram_tensor(name, list(shape), dt, kind=kind).ap()

    def _collect(self, reads, writes):
        deps = {}
        for r in reads:
            if r.lw is not None:
                e, s = r.lw
                if deps.get(e.name, (None, 0))[1] < s:
                    deps[e.name] = (e, s)
        for w in writes:
            if w.lw is not None:
                e, s = w.lw
                if deps.get(e.name, (None, 0))[1] < s:
                    deps[e.name] = (e, s)
            for (e, s) in w.rd:
                if deps.get(e.name, (None, 0))[1] < s:
                    deps[e.name] = (e, s)
        return deps

    def _wait_deps(self, E, deps):
        for name, (e, s) in deps.items():
            if e is E and not E.self_sync:
                continue
            if E.clock.get(name, 0) >= s:
                continue
            sem, val = e.sem_for(s)
            E.h.wait_ge(sem, val)
            self.nwaits += 1
            oc = e.op_clocks[s]
            for k, v in oc.items():
                if E.clock.get(k, 0) < v:
                    E.clock[k] = v
            if E.clock.get(name, 0) < s:
                E.clock[name] = s

    def _commit(self, E, seq, reads, writes):
        for w in writes:
            w.lw = (E, seq)
            w.rd = []
        for r in reads:
            r.rd.append((E, seq))

    def op(self, E, fn, reads=(), writes=()):
        self._wait_deps(E, self._collect(reads, writes))
        ins = fn()
        E.count += 1
        seq = E.count
        sem, val = E.sem_for(seq)
        ins.then_inc(sem, 1)
        E.clock[E.name] = seq if not E.self_sync else E.clock.get(E.name, 0)
        ck = dict(E.clock)
        ck[E.name] = seq
        E.op_clocks.append(ck)
        self._commit(E, seq, reads, writes)
        return ins

    def group(self, E, fns, reads=(), writes=()):
        self._wait_deps(E, self._collect(reads, writes))
        ins = None
        for fn in fns:
            ins = fn()
        E.count += 1
        seq = E.count
        sem, val = E.sem_for(seq)
        ins.then_inc(sem, 1)
        E.clock[E.name] = seq if not E.self_sync else E.clock.get(E.name, 0)
        ck = dict(E.clock)
        ck[E.name] = seq
        E.op_clocks.append(ck)
        self._commit(E, seq, reads, writes)
        return ins

    def dma(self, Q, out, in_, reads=(), writes=(), **kw):
        d = self.dmas[self.dma_rr]
        self.dma_rr = (self.dma_rr + 1) % self.ndma
        deps = self._collect(reads, writes)
        if d.count > 0:
            if deps.get(d.name, (None, 0))[1] < d.count:
                deps[d.name] = (d, d.count)
        self._wait_deps(Q, deps)
        ins = Q.h.dma_start(out=out, in_=in_, **kw)
        d.count += 1
        seq = d.count
        sem, val = d.sem_for(seq)
        ins.then_inc(sem, 16)
        ck = dict(Q.clock)
        ck[d.name] = seq
        d.op_clocks.append(ck)
        self._commit(d, seq, reads, writes)
        return ins

    def wait_all(self, E, regs):
        deps = self._collect(regs, ())
        self._wait_deps(E, deps)

    def barrier(self):
        for E in (self.pe, self.act, self.dve, self.pool, self.sp):
            deps = {}
            for e in self.engs.values():
                if e.count > 0:
                    deps[e.name] = (e, e.count)
            ss = E.self_sync
            E.self_sync = True
            self._wait_deps(E, deps)
            E.self_sync = ss

    def scope(self):
        return Scope(self)

    def close(self):
        self.es.close()


class Scope:
    def __init__(self, fw):
        self.fw = fw

    def __enter__(self):
        self.saved = self.fw.es
        self.fw.es = ExitStack()
        return self

    def __exit__(self, *a):
        self.fw.barrier()
        self.fw.es.close()
        self.fw.es = self.saved
        return False


class Ring:
    def __init__(self, fw, name, shape, dt, n, psum=False):
        self.tiles = []
        self.regs = []
        for i in range(n):
            t = fw.ps(f"{name}{i}", shape, dt) if psum else fw.sb(f"{name}{i}", shape, dt)
            self.tiles.append(t)
            self.regs.append(Reg(f"{name}{i}"))
        self.i = 0
        self.n = n

    def next(self):
        t, r = self.tiles[self.i], self.regs[self.i]
        self.i = (self.i + 1) % self.n
        return t, r

from concourse.bass_utils import run_bass_kernel_spmd

D = 2048
S = 2048
L = 2
FF = 5504
NFC = FF // 128
W = 1024
KC = D // 128
TB = 512
NTB = S // TB
EPS = 1e-6
NMOD = 9
FPARTS = [(0, 11), (11, 11), (22, 11), (33, 10)]


class KB:
    def __init__(self, nc, stages):
        self.nc = nc
        self.fw = FW(nc)
        self.stages = stages
        fw = self.fw
        dr = fw.dram
        self.xT_in = dr("xT", [D, S], F32, "ExternalInput")
        self.cT = dr("cT", [128, KC], F32, "ExternalInput")
        self.ada_w = [dr(f"ada_w{l}", [D, NMOD * D], F32, "ExternalInput") for l in range(L)]
        self.ada_bT = [dr(f"ada_bT{l}", [128, NMOD * KC], F32, "ExternalInput") for l in range(L)]
        self.norm_wT = [dr(f"norm_wT{l}", [128, 3 * KC], F32, "ExternalInput") for l in range(L)]
        self.fnorm_wT = dr("fnorm_wT", [128, KC], F32, "ExternalInput")
        self.w1 = [[dr(f"w1_{l}_{i}", [D, FF], F32, "ExternalInput") for i in range(2)] for l in range(L)]
        self.w3 = [[dr(f"w3_{l}_{i}", [D, FF], F32, "ExternalInput") for i in range(2)] for l in range(L)]
        self.w2 = [[dr(f"w2_{l}_{i}", [FF, D], F32, "ExternalInput") for i in range(2)] for l in range(L)]
        self.outT = dr("outT", [D, S], F32, "ExternalOutput")
        self.xres = dr("xres", [D, S], F32, "Internal")
        self.x_regs = [[Reg(f"x_{dc}_{tb}") for tb in range(NTB)] for dc in range(KC)]
        self.x_cur = self.xT_in
        self.ones_bf = fw.sb("ones_bf", [128, 128], BF16)
        self.ones_r = Reg("ones_bf")
        self.eps_t = fw.sb("eps_t", [128, 1], F32)
        self.eps_r = Reg("eps")
        fw.op(fw.dve, lambda: nc.vector.memset(self.ones_bf[:], 1.0), writes=[self.ones_r])
        fw.op(fw.dve, lambda: nc.vector.memset(self.eps_t[:], EPS), writes=[self.eps_r])
        self.banks = [fw.ps(f"bank{i}", [128, 512], F32) for i in range(8)]
        self.bank_r = [Reg(f"bank{i}") for i in range(8)]
        self.mod = [fw.sb(f"mod{l}", [128, NMOD * KC], F32) for l in range(L)]
        self.mod_r = [Reg(f"mod{l}") for l in range(L)]
        self.avec = fw.sb("avec", [128, L * 3 * KC], F32)
        self.gvec = fw.sb("gvec", [128, L * 3 * KC], F32)
        self.av_r = Reg("avec")
        self.gv_r = Reg("gvec")
        self.fnw = fw.sb("fnw", [128, KC], F32)
        self.fnw_r = Reg("fnw")
        self.hT = fw.sb("hT", [128, KC, S], BF16)
        self.hT_r = [[Reg(f"hT_{c}_{tb}") for tb in range(NTB)] for c in range(KC)]

    def alloc_persistent(self):
        fw = self.fw
        self.cact = fw.sb("cact", [128, KC], F32)
        self.cact_r = Reg("cact")
        self.cact_bf = fw.sb("cact_bf", [128, KC], BF16)

    def prologue(self):
        nc, fw = self.nc, self.fw
        c_sb = fw.sb("c_sb", [128, KC], F32)
        c_r = Reg("c_sb")
        fw.dma(fw.sp, c_sb[:], self.cT[:, :], writes=[c_r])
        fw.op(fw.act, lambda: nc.scalar.activation(out=self.cact[:], in_=c_sb[:], func=AF.Silu),
              reads=[c_r], writes=[self.cact_r])
        fw.op(fw.act, lambda: nc.scalar.activation(out=self.cact_bf[:], in_=c_sb[:], func=AF.Silu),
              reads=[c_r], writes=[self.cact_r])
        fw.dma(fw.sp, self.fnw[:], self.fnorm_wT[:, :], writes=[self.fnw_r])

    def prologue_extra(self):
        pass

    def mod_gen(self, l, bank_i):
        nc, fw = self.nc, self.fw
        cact, cact_r = self.cact, self.cact_r
        NB = 256
        ring = Ring(fw, "adaw", [128, KC, NB], BF16, 2)
        cact = self.cact_bf
        adab = fw.sb("adab", [128, NMOD * KC], F32)
        adab_r = Reg("adab")
        nw = fw.sb("nw", [128, 3 * KC], F32)
        nw_r = Reg("nw")
        fw.dma(fw.sp, adab[:], self.ada_bT[l][:, :], writes=[adab_r])
        fw.dma(fw.sp, nw[:], self.norm_wT[l][:, :], writes=[nw_r])
        if True:
            bank, bank_r = self.banks[bank_i], self.bank_r[bank_i]
            aw = self.ada_w[l].rearrange("(kc p) n -> p kc n", p=128)
            NT = NMOD * D // NB
            tl = {}

            def issue(nb):
                t, r = ring.next()
                fw.dma(fw.pool, t[:], aw[:, :, nb * NB:(nb + 1) * NB], writes=[r])
                tl[nb] = (t, r)
            issue(0)
            for nb in range(NT):
                if nb + 1 < NT:
                    issue(nb + 1)
                t, r = tl.pop(nb)
                for j in range(NB // 128):
                    col = nb * (NB // 128) + j
                    fns = []
                    for kc in range(KC):
                        fns.append(lambda kc=kc, j=j, col=col, t=t: nc.tensor.matmul(
                            bank[:, col:col + 1], lhsT=t[:, kc, j * 128:(j + 1) * 128],
                            rhs=cact[:, kc:kc + 1], start=(kc == 0), stop=(kc == KC - 1)))
                    fw.group(fw.pe, fns, reads=[r, cact_r], writes=[bank_r])
                if (nb + 1) % 24 == 0:
                    s = nb // 24
                    c0, c1 = 48 * s, 48 * (s + 1)
                    fw.op(fw.dve, lambda: nc.vector.tensor_tensor(
                        out=self.mod[l][:, c0:c1], in0=bank[:, c0:c1], in1=adab[:, c0:c1], op=ALU.add),
                        reads=[bank_r, adab_r], writes=[self.mod_r[l]])
                    sc = self.mod[l][:, (3 * s + 1) * KC:(3 * s + 2) * KC]
                    gt = self.mod[l][:, (3 * s + 2) * KC:(3 * s + 3) * KC]
                    o = (l * 3 + s) * KC
                    fw.op(fw.dve, lambda: nc.vector.scalar_tensor_tensor(
                        out=self.avec[:, o:o + KC], in0=sc, scalar=1.0, in1=nw[:, s * KC:(s + 1) * KC],
                        op0=ALU.add, op1=ALU.mult), reads=[self.mod_r[l], nw_r], writes=[self.av_r])
                    gm = 1.0 if s == 1 else 0.5
                    fw.op(fw.dve, lambda: nc.vector.tensor_scalar(
                        out=self.gvec[:, o:o + KC], in0=gt, scalar1=gm, scalar2=None, op0=ALU.mult),
                        reads=[self.mod_r[l]], writes=[self.gv_r])
                yield

    def norm_phase(self, a_ap, b_ap, a_regs, out_kind="hT", out_dram=None):
        nc, fw = self.nc, self.fw
        NB = 256
        self.xblk_ring = Ring(fw, "xblk", [128, KC, NB], F32, 2)
        self.sq_ring = Ring(fw, "sq", [128, KC, NB], BF16, 2)
        self.rs_ring = Ring(fw, "rs", [128, NB], F32, 2)
        self.rstd_ring = Ring(fw, "rstd", [128, NB], F32, 2)
        self.tmp_ring = Ring(fw, "ntmp", [128, NB], F32, 3)
        xsrc = self.x_cur.rearrange("(c p) t -> p c t", p=128)
        for tb8 in range(S // NB):
            ts = slice(tb8 * NB, (tb8 + 1) * NB)
            tb = tb8 // 2
            xb, xb_r = self.xblk_ring.next()
            fw.dma(fw.sp, xb[:], xsrc[:, :, ts], reads=[self.x_regs[c][tb] for c in range(KC)], writes=[xb_r])
            sq, sq_r = self.sq_ring.next()
            fw.op(fw.act, lambda: nc.scalar.activation(out=sq[:], in_=xb[:], func=AF.Square),
                  reads=[xb_r], writes=[sq_r])
            bank, bank_r = self.banks[0], self.bank_r[0]
            fns = [lambda c=c: nc.tensor.matmul(bank[:, 0:NB], lhsT=self.ones_bf[:], rhs=sq[:, c, :],
                                                start=(c == 0), stop=(c == KC - 1)) for c in range(KC)]
            fw.group(fw.pe, fns, reads=[sq_r, self.ones_r], writes=[bank_r])
            rs, rs_r = self.rs_ring.next()
            fw.op(fw.act, lambda: nc.scalar.activation(out=rs[:], in_=bank[:, 0:NB], func=AF.Sqrt,
                                                       bias=self.eps_t[:], scale=1.0 / D),
                  reads=[bank_r, self.eps_r], writes=[rs_r])
            rstd, rstd_r = self.rstd_ring.next()
            fw.op(fw.dve, lambda: nc.vector.reciprocal(out=rstd[:], in_=rs[:]), reads=[rs_r], writes=[rstd_r])
            for c in range(KC):
                tmp, tmp_r = self.tmp_ring.next()
                fw.op(fw.dve, lambda c=c, tmp=tmp: nc.vector.scalar_tensor_tensor(
                    out=tmp[:], in0=xb[:, c, :], scalar=a_ap[:, c:c + 1], in1=rstd[:],
                    op0=ALU.mult, op1=ALU.mult), reads=[xb_r, rstd_r] + a_regs, writes=[tmp_r])
                if out_kind == "hT":
                    fw.op(fw.act, lambda c=c, tmp=tmp: nc.scalar.activation(
                        out=self.hT[:, c, ts], in_=tmp[:], func=AF.Identity, bias=b_ap[:, c:c + 1], scale=1.0),
                        reads=[tmp_r] + a_regs, writes=[self.hT_r[c][tb]])
                else:
                    fw.dma(fw.sp, out_dram[c * 128:(c + 1) * 128, ts], tmp[:], reads=[tmp_r],
                           writes=[self.out_regs[c][tb]])

    def ffn(self, l, i):
        with self.fw.scope():
            self._ffn(l, i)

    def _ffn(self, l, i):
        nc, fw = self.nc, self.fw
        s = 0 if i == 0 else 2
        o = (l * 3 + s) * KC
        a_ap = self.avec[:, o:o + KC]
        b_ap = self.mod[l][:, (3 * s) * KC:(3 * s + 1) * KC]
        g_ap = self.gvec[:, o:o + KC]
        with fw.scope():
            self.norm_phase(a_ap, b_ap, [self.av_r, self.mod_r[l]])
        if True:
            self.w1_ring = Ring(fw, "w1t", [128, KC, 256], BF16, 2)
            self.w3_ring = Ring(fw, "w3t", [128, KC, 256], BF16, 2)
            self.w2_ring = Ring(fw, "w2t", [128, 11, 512], BF16, 2)
            self.w13_ring = True
            self.GT = fw.sb("GT", [128, 11, S], BF16)
            self.GT_r = [[Reg(f"GT_{f}_{tb}") for tb in range(NTB)] for f in range(11)]
            self.sil_ring = Ring(fw, "sil", [128, TB], F32, 3)
            self.xo_ring = Ring(fw, "xo", [128, TB], F32, 6)
            self.abank_i = 0
            self.bbank_i = 0
        w1 = self.w1[l][i].rearrange("(kc p) f -> p kc f", p=128)
        w3 = self.w3[l][i].rearrange("(kc p) f -> p kc f", p=128)
        w2 = self.w2[l][i].rearrange("(fc p) d -> p fc d", p=128)
        for (c0, n) in FPARTS:
            blocks = []
            c = c0
            while c < c0 + n:
                nb = min(2, c0 + n - c)
                blocks.append((c, nb))
                c += nb
            for (bc, nb) in blocks:
                mgl = getattr(self, "mg_live", None)
                if mgl is not None:
                    for _ in range(2):
                        try:
                            next(mgl)
                        except StopIteration:
                            self.mg_live = None
                            break
                w1t, w1r = self.w1_ring.next()
                w3t, w3r = self.w3_ring.next()
                fs = slice(bc * 128, (bc + nb) * 128)
                fw.dma(fw.pool, w1t[:, :, 0:nb * 128], w1[:, :, fs], writes=[w1r])
                fw.dma(fw.pool, w3t[:, :, 0:nb * 128], w3[:, :, fs], writes=[w3r])
                for fi in range(nb):
                    fcl = bc + fi - c0
                    for tb in range(NTB):
                        ts = slice(tb * TB, (tb + 1) * TB)
                        ia = (self.abank_i % 2) * 2
                        self.abank_i += 1
                        ba, ba_r = self.banks[ia], self.bank_r[ia]
                        bb, bb_r = self.banks[ia + 1], self.bank_r[ia + 1]
                        hregs = [self.hT_r[kc][tb] for kc in range(KC)]
                        fns = [lambda kc=kc, w1t=w1t, fi=fi, ba=ba, ts=ts: nc.tensor.matmul(
                            ba[:], lhsT=w1t[:, kc, fi * 128:(fi + 1) * 128], rhs=self.hT[:, kc, ts],
                            start=(kc == 0), stop=(kc == KC - 1)) for kc in range(KC)]
                        fw.group(fw.pe, fns, reads=[w1r] + hregs, writes=[ba_r])
                        fns = [lambda kc=kc, w3t=w3t, fi=fi, bb=bb, ts=ts: nc.tensor.matmul(
                            bb[:], lhsT=w3t[:, kc, fi * 128:(fi + 1) * 128], rhs=self.hT[:, kc, ts],
                            start=(kc == 0), stop=(kc == KC - 1)) for kc in range(KC)]
                        fw.group(fw.pe, fns, reads=[w3r] + hregs, writes=[bb_r])
                        sil, sil_r = self.sil_ring.next()
                        fw.op(fw.act, lambda sil=sil, ba=ba: nc.scalar.activation(out=sil[:], in_=ba[:], func=AF.Silu),
                              reads=[ba_r], writes=[sil_r])
                        fw.op(fw.dve, lambda sil=sil, bb=bb, fcl=fcl, ts=ts: nc.vector.tensor_tensor(
                            out=self.GT[:, fcl, ts], in0=sil[:], in1=bb[:], op=ALU.mult),
                            reads=[sil_r, bb_r], writes=[self.GT_r[fcl][tb]])
            xsrc = self.x_cur
            for dq in range(4):
                w2t, w2r = self.w2_ring.next()
                fw.dma(fw.pool, w2t[:, 0:n, :], w2[:, c0:c0 + n, dq * 512:(dq + 1) * 512], writes=[w2r])
                for dci in range(4):
                    dc = dq * 4 + dci
                    for tb in range(NTB):
                        ts = slice(tb * TB, (tb + 1) * TB)
                        ib = 4 + (self.bbank_i % (3 if getattr(self, "mg_live", None) is not None else 4))
                        self.bbank_i += 1
                        bk, bk_r = self.banks[ib], self.bank_r[ib]
                        xo, xo_r = self.xo_ring.next()
                        fw.dma(fw.sp, xo[:], xsrc[dc * 128:(dc + 1) * 128, ts], reads=[self.x_regs[dc][tb]], writes=[xo_r])
                        fns = [lambda f=f, w2t=w2t, dci=dci, bk=bk, ts=ts: nc.tensor.matmul(
                            bk[:], lhsT=w2t[:, f, dci * 128:(dci + 1) * 128], rhs=self.GT[:, f, ts],
                            start=(f == 0), stop=(f == n - 1)) for f in range(n)]
                        fw.group(fw.pe, fns, reads=[w2r] + [self.GT_r[f][tb] for f in range(n)], writes=[bk_r])
                        fw.op(fw.dve, lambda bk=bk, xo=xo, dc=dc: nc.vector.scalar_tensor_tensor(
                            out=xo[:], in0=bk[:], scalar=g_ap[:, dc:dc + 1], in1=xo[:], op0=ALU.mult, op1=ALU.add),
                            reads=[bk_r, xo_r, self.gv_r], writes=[xo_r])
                        fw.dma(fw.act, self.xres[dc * 128:(dc + 1) * 128, ts], xo[:], reads=[xo_r],
                               writes=[self.x_regs[dc][tb]])
            self.x_cur = self.xres

    def final(self):
        with self.fw.scope():
            self._final()

    def _final(self):
        nc, fw = self.nc, self.fw
        self.out_regs = [[Reg(f"o_{c}_{tb}") for tb in range(NTB)] for c in range(KC)]
        self.norm_phase(self.fnw, None, [self.fnw_r], out_kind="out", out_dram=self.outT)
        allr = [r for rr in self.out_regs for r in rr]
        fw.wait_all(fw.sp, allr)

    def build(self):
        self.prologue()
        for st in self.stages:
            if st[0] == "ffn":
                self.ffn(st[1], st[2])
        self.final()
        self.fw.close()


WIN = 14344
SBK = [0, 1, 6, 7]
OFF = dict(qa=0, ka=1024, va=2048, fa=3072, us=3080, qr=4104, kr=5128, vr=6152, gr=7176, gl=8200)
HD = 128
NH = 8
QS = HD ** -0.5


class KBM(KB):
    def __init__(self, nc, stages, debug=False):
        super().__init__(nc, stages)
        fw = self.fw
        dr = fw.dram
        self.debug = debug
        self.w_in = [dr(f"w_in{l}", [D, WIN], F32, "ExternalInput") for l in range(L)]
        self.fbias = [dr(f"fbias{l}", [8, 1], F32, "ExternalInput") for l in range(L)]
        self.glu_w = [dr(f"glu_w{l}", [W, W], F32, "ExternalInput") for l in range(L)]
        self.glu_bT = [dr(f"glu_bT{l}", [128, 8], F32, "ExternalInput") for l in range(L)]
        self.gn_wT = [dr(f"gn_wT{l}", [128, 8], F32, "ExternalInput") for l in range(L)]
        self.w_br = [dr(f"w_br{l}", [3 * W, D], F32, "ExternalInput") for l in range(L)]
        self.b_gateT = [dr(f"b_gateT{l}", [128, 3 * KC], F32, "ExternalInput") for l in range(L)]
        self.w_out = [dr(f"w_out{l}", [D, D], F32, "ExternalInput") for l in range(L)]
        self.sA_reS = [dr(f"sA_reS{l}", [128, 32], F32, "ExternalInput") for l in range(L)]
        self.sA_imS = [dr(f"sA_imS{l}", [128, 32], F32, "ExternalInput") for l in range(L)]
        self.sldtS = [dr(f"sldtS{l}", [128, 32], F32, "ExternalInput") for l in range(L)]
        self.sA_reR = [dr(f"sA_reR{l}", [128, 512], F32, "ExternalInput") for l in range(L)]
        self.sA_imR = [dr(f"sA_imR{l}", [128, 512], F32, "ExternalInput") for l in range(L)]
        self.sldtR = [dr(f"sldtR{l}", [128, 512], F32, "ExternalInput") for l in range(L)]
        self.sB_reR = [dr(f"sB_reR{l}", [128, 512], F32, "ExternalInput") for l in range(L)]
        self.sB_imR = [dr(f"sB_imR{l}", [128, 512], F32, "ExternalInput") for l in range(L)]
        self.sC_reS = [dr(f"sC_reS{l}", [128, 512], F32, "ExternalInput") for l in range(L)]
        self.sC_imS = [dr(f"sC_imS{l}", [128, 512], F32, "ExternalInput") for l in range(L)]
        self.sD_T = [dr(f"sD_T{l}", [128, 8], F32, "ExternalInput") for l in range(L)]
        self.c_tri = dr("c_tri", [128, 128], F32, "ExternalInput")
        self.c_rotC = dr("c_rotC", [128, S], F32, "ExternalInput")
        self.c_rotS = dr("c_rotS", [128, S], F32, "ExternalInput")
        self.c_gfull = dr("c_gfull", [8, 128, 512], F32, "ExternalInput")
        self.c_ttri = dr("c_ttri", [8, 128, 512], F32, "ExternalInput")
        self.c_mrow = dr("c_mrow", [128, 8], F32, "ExternalInput")
        self.c_mst = dr("c_mst", [128, 2], F32, "ExternalInput")
        kind = "ExternalOutput" if debug else "Internal"
        self.yf_d = dr("yf_d", [W, S], BF16, kind)
        self.ys_d = dr("ys_d", [W, S], BF16, kind)
        self.yr_d = dr("yr_d", [W, S], BF16, kind)
        self.y_regs = {n: [[Reg(f"{n}_{c}_{tb}") for tb in range(NTB)] for c in range(8)] for n in ("yf", "ys", "yr")}
        self.sst_d = []
        self.sst_r = []
        for l in range(L):
            self.sst_d.append({"rS": dr(f"sst_rS{l}", [128, 32], F32, "Internal"),
                               "cosT": dr(f"sst_cos{l}", [128, 4096], F32, "Internal"),
                               "sinT": dr(f"sst_sin{l}", [128, 4096], F32, "Internal"),
                               "BD0": dr(f"sst_bd0{l}", [128, 4096], BF16, "Internal"),
                               "BD1": dr(f"sst_bd1{l}", [128, 4096], BF16, "Internal"),
                               "CBD0": dr(f"sst_cbd0{l}", [128, 1024], BF16, "Internal"),
                               "CBD1": dr(f"sst_cbd1{l}", [128, 1024], BF16, "Internal")})
            self.sst_r.append(Reg(f"sst{l}"))
        self.pd = {}
        self.pd_r = {}
        for n, shp, dt in (("qa", [W, S], BF16), ("ka", [W, S], BF16), ("va", [S, W], BF16), ("vr", [S, W], BF16),
                           ("qr", [W, S], F32), ("kr", [W, S], F32), ("gr", [W, S], F32), ("g", [W, S], BF16)):
            self.pd[n] = dr(n + "_d", shp, dt, "Internal")
            if n in ("va", "vr"):
                self.pd_r[n] = [Reg(f"{n}_d{t}") for t in range(16)]
            else:
                self.pd_r[n] = [[Reg(f"{n}_d{c}_{tb}") for tb in range(NTB)] for c in range(8)]
        self.ones_f = fw.sb("ones_f", [128, 1], F32)
        self.ones_fr = Reg("ones_f")
        fw.op(fw.dve, lambda: nc.vector.memset(self.ones_f[:], 1.0), writes=[self.ones_fr])

    def prologue_extra(self):
        self.sst_done = set()
        ls = sorted({st[1] for st in self.stages if st[0] == "mix" and (len(st) < 3 or "ssm" in st[2])})
        if ls and self.sst_in_prologue:
            self.ssm_precompute(ls[0])

    def win_cols(self, l, col0, ncols):
        return self.w_in[l].rearrange("(kc p) n -> p kc n", p=128)[:, :, col0:col0 + ncols]

    def projA(self, wsrc, ncols, K, rhs_fn, rhs_regs, consume, banks, ring, Mchunk=128):
        nc, fw = self.nc, self.fw
        c = 0
        ci = 0
        while c < ncols:
            nt = min(256, ncols - c)
            wt, wr = ring.next()
            fw.dma(fw.pool, wt[:, 0:K, 0:nt], wsrc[:, :, c:c + nt], writes=[wr])
            for j in range(0, nt, Mchunk):
                m = min(Mchunk, nt - j)
                for tb in range(NTB):
                    ts = slice(tb * TB, (tb + 1) * TB)
                    bi = banks[self._bk % len(banks)]
                    self._bk += 1
                    bk, bk_r = self.banks[bi], self.bank_r[bi]
                    fns = [lambda kc=kc: nc.tensor.matmul(bk[0:m, :], lhsT=wt[:, kc, j:j + m], rhs=rhs_fn(kc, ts),
                                                         start=(kc == 0), stop=(kc == K - 1)) for kc in range(K)]
                    fw.group(fw.pe, fns, reads=[wr] + rhs_regs(tb), writes=[bk_r])
                    consume(bk, bk_r, ci, tb, m)
                ci += 1
            c += nt

    def projA_gen(self, wsrc, ncols, K, rhs_fn, rhs_regs, consume, banks, ring):
        nc, fw = self.nc, self.fw
        c = 0
        ci = 0
        while c < ncols:
            nt = min(256, ncols - c)
            wt, wr = ring.next()
            fw.dma(fw.pool, wt[:, 0:K, 0:nt], wsrc[:, :, c:c + nt], writes=[wr])
            for j in range(0, nt, 128):
                m = min(128, nt - j)
                for tb in range(NTB):
                    ts = slice(tb * TB, (tb + 1) * TB)
                    bi = banks[self._bk % len(banks)]
                    self._bk += 1
                    bk, bk_r = self.banks[bi], self.bank_r[bi]
                    fns = [lambda kc=kc: nc.tensor.matmul(bk[0:m, :], lhsT=wt[:, kc, j:j + m], rhs=rhs_fn(kc, ts),
                                                         start=(kc == 0), stop=(kc == K - 1)) for kc in range(K)]
                    fw.group(fw.pe, fns, reads=[wr] + rhs_regs(tb), writes=[bk_r])
                    consume(bk, bk_r, ci, tb, m)
                    yield
                ci += 1
            c += nt

    def projB_gen(self, l, col0, ncols, dst, dst_r, banks, ring, st_ring):
        nc, fw = self.nc, self.fw
        wsrc = self.win_cols(l, col0, ncols)
        for c in range(0, ncols, 256):
            wt, wr = ring.next()
            fw.dma(fw.pool, wt[:, :, 0:256], wsrc[:, :, c:c + 256], writes=[wr])
            for tkb in range(16):
                tb = tkb // 4
                bi = banks[self._bk % len(banks)]
                self._bk += 1
                bk, bk_r = self.banks[bi], self.bank_r[bi]
                fns = [lambda kc=kc: nc.tensor.matmul(bk[:, 0:256], lhsT=self.hT[:, kc, tkb * 128:(tkb + 1) * 128],
                                                     rhs=wt[:, kc, 0:256], start=(kc == 0), stop=(kc == KC - 1))
                       for kc in range(KC)]
                fw.group(fw.pe, fns, reads=[wr] + self.h_regs(tb), writes=[bk_r])
                st, st_r = st_ring.next()
                fw.op(fw.act, lambda: nc.scalar.activation(out=st[:], in_=bk[:, 0:256], func=AF.Copy), reads=[bk_r], writes=[st_r])
                fw.dma(fw.sp, dst[tkb * 128:(tkb + 1) * 128, c:c + 256], st[:], reads=[st_r], writes=[dst_r[tkb]])
                yield

    def proj_items(self, l):
        nc, fw = self.nc, self.fw
        wring = Ring(fw, "wA", [128, KC, 256], BF16, 2)
        stb = Ring(fw, "pstb", [128, TB], BF16, 3)
        stf = Ring(fw, "pstf", [128, TB], F32, 2)
        stv = Ring(fw, "pstv", [128, 256], BF16, 2)

        def mk(name, func, scale, ring):
            def cons(bk, bk_r, ci, tb, m):
                st, st_r = ring.next()
                fw.op(fw.act, lambda: nc.scalar.activation(out=st[:], in_=bk[:], func=func, scale=scale), reads=[bk_r], writes=[st_r])
                fw.dma(fw.sp, self.pd[name][ci * 128:(ci + 1) * 128, tb * TB:(tb + 1) * TB], st[:], reads=[st_r],
                       writes=[self.pd_r[name][ci][tb]])
            return cons
        A = lambda name, func, scale, ring: self.projA_gen(self.win_cols(l, OFF[name], W), W, KC, self.h_rhs, self.h_regs,
                                                            mk(name, func, scale, ring), [6, 7], wring)
        yield from A("qa", AF.Copy, QS, stb)
        yield from A("ka", AF.Copy, 1.0, stb)
        yield from self.projB_gen(l, OFF["va"], W, self.pd["va"], self.pd_r["va"], [6, 7], wring, stv)
        yield from A("qr", AF.Copy, 1.0, stf)
        yield from A("kr", AF.Copy, 1.0, stf)
        yield from self.projB_gen(l, OFF["vr"], W, self.pd["vr"], self.pd_r["vr"], [6, 7], wring, stv)
        yield from A("gr", AF.Silu, 1.0, stf)

    def h_rhs(self, kc, ts):
        return self.hT[:, kc, ts]

    def h_regs(self, tb):
        return [self.hT_r[kc][tb] for kc in range(KC)]

    def projB(self, l, col0, ncols, vtm, vtm_r, banks, ring):
        nc, fw = self.nc, self.fw
        wsrc = self.win_cols(l, col0, ncols)
        for c in range(0, ncols, 256):
            wt, wr = ring.next()
            fw.dma(fw.pool, wt[:, :, 0:256], wsrc[:, :, c:c + 256], writes=[wr])
            for tkb in range(16):
                tb = tkb // 4
                bi = banks[self._bk % len(banks)]
                self._bk += 1
                bk, bk_r = self.banks[bi], self.bank_r[bi]
                fns = [lambda kc=kc: nc.tensor.matmul(bk[:, 0:256], lhsT=self.hT[:, kc, tkb * 128:(tkb + 1) * 128],
                                                     rhs=wt[:, kc, 0:256], start=(kc == 0), stop=(kc == KC - 1))
                       for kc in range(KC)]
                fw.group(fw.pe, fns, reads=[wr] + self.h_regs(tb), writes=[bk_r])
                fw.op(fw.act, lambda: nc.scalar.activation(out=vtm[:, tkb, c:c + 256], in_=bk[:, 0:256], func=AF.Copy),
                      reads=[bk_r], writes=[vtm_r[tkb]])

    def mixer(self, l):
        nc, fw = self.nc, self.fw
        s = 1
        o = (l * 3 + s) * KC
        a_ap = self.avec[:, o:o + KC]
        b_ap = self.mod[l][:, (3 * s) * KC:(3 * s + 1) * KC]
        self._bk = 0
        with fw.scope():
            self.norm_phase(a_ap, b_ap, [self.av_r, self.mod_r[l]])
        if "ssm" in self.parts:
            with fw.scope():
                self.ssm(l)
        if "fox" in self.parts:
            with fw.scope():
                self.fox(l)
        if "ret" in self.parts:
            with fw.scope():
                self.ret(l)
        if "merge" in self.parts:
            with fw.scope():
                self.merge(l)

    def fox(self, l):
        nc, fw = self.nc, self.fw
        vtm = fw.sb("vtm", [128, 16, W], BF16)
        vtm_r = [Reg(f"vtm{t}") for t in range(16)]
        kaug = [fw.sb(f"kaug{t}", [128, S], BF16) for t in range(2)]
        qaug = [fw.sb(f"qaug{t}", [128, S], BF16) for t in range(2)]
        kaug_r = [Reg(f"kaug{t}") for t in range(2)]
        qaug_r = [Reg(f"qaug{t}") for t in range(2)]
        tri = fw.sb("tri", [128, 128], BF16)
        tri_r = Reg("tri")
        with fw.scope():
            trif = fw.sb("trif", [128, 128], F32)
            trif_r = Reg("trif")
            fw.dma(fw.sp, trif[:], self.c_tri[:, :], writes=[trif_r])
            fw.op(fw.dve, lambda: nc.vector.tensor_copy(out=tri[:], in_=trif[:]), reads=[trif_r], writes=[tri_r])
            for t in range(2):
                fw.op(fw.dve, lambda t=t: nc.vector.memset(kaug[t][:], 1.0), writes=[kaug_r[t]])
                fw.op(fw.dve, lambda t=t: nc.vector.memset(qaug[t][:], 1.0), writes=[qaug_r[t]])
            wfa = fw.sb("wfa", [128, KC, 8], BF16)
            wfa_r = Reg("wfa")
            fw.dma(fw.pool, wfa[:], self.win_cols(l, OFF["fa"], 8), writes=[wfa_r])
            fb = fw.sb("fb", [8, 1], F32)
            fb_r = Reg("fb")
            nfb = fw.sb("nfb", [8, 1], F32)
            nfb_r = Reg("nfb")
            fw.dma(fw.sp, fb[:], self.fbias[l][:, :], writes=[fb_r])
            fw.op(fw.dve, lambda: nc.vector.tensor_scalar(out=nfb[:], in0=fb[:], scalar1=-1.0, scalar2=None, op0=ALU.mult),
                  reads=[fb_r], writes=[nfb_r])
            e_t = fw.sb("e_t", [8, S], F32)
            e_r = Reg("e_t")
            for tb in range(NTB):
                ts = slice(tb * TB, (tb + 1) * TB)
                bk, bk_r = self.banks[6 + tb % 2], self.bank_r[6 + tb % 2]
                fns = [lambda kc=kc: nc.tensor.matmul(bk[0:8, :], lhsT=wfa[:, kc, 0:8], rhs=self.hT[:, kc, ts],
                                                     start=(kc == 0), stop=(kc == KC - 1)) for kc in range(KC)]
                fw.group(fw.pe, fns, reads=[wfa_r] + self.h_regs(tb), writes=[bk_r])
                fw.op(fw.act, lambda: nc.scalar.activation(out=e_t[:, ts], in_=bk[0:8, :], func=AF.Exp,
                                                           bias=nfb[:], scale=-1.0),
                      reads=[bk_r, nfb_r], writes=[e_r])
            lf = fw.sb("lf", [8, S], F32)
            lf_r = Reg("lf")
            fw.op(fw.act, lambda: nc.scalar.activation(out=lf[:], in_=e_t[:], func=AF.Ln, bias=self.ones_f[0:8, :], scale=1.0),
                  reads=[e_r, self.ones_fr], writes=[lf_r])
            G = fw.sb("G", [8, S], F32)
            G_r = Reg("G")
            fw.op(fw.dve, lambda: nc.vector.tensor_tensor_scan(
                out=G[:], data0=self.ones_f[0:8, 0:1].to_broadcast([8, S]), data1=lf[:], initial=0.0,
                op0=ALU.mult, op1=ALU.add), reads=[lf_r, self.ones_fr], writes=[G_r])
            Gs = [fw.sb(f"G{i}", [8, S], BF16) for i in range(3)]
            NGs = [fw.sb(f"NG{i}", [8, S], BF16) for i in range(3)]
            Gs_r = [Reg(f"G{i}") for i in range(3)]
            NGs_r = [Reg(f"NG{i}") for i in range(3)]
            R = fw.sb("Rres", [8, S], F32)
            R_r = Reg("Rres")
            cur, cur_r = G, G_r
            for i in range(3):
                fw.op(fw.dve, lambda i=i, cur=cur: nc.vector.tensor_copy(out=Gs[i][:], in_=cur[:]), reads=[cur_r], writes=[Gs_r[i]])
                fw.op(fw.dve, lambda i=i: nc.vector.tensor_scalar(out=NGs[i][:], in0=Gs[i][:], scalar1=-1.0, scalar2=None, op0=ALU.mult),
                      reads=[Gs_r[i]], writes=[NGs_r[i]])
                if i < 2:
                    fw.op(fw.dve, lambda i=i, cur=cur: nc.vector.tensor_tensor(out=R[:], in0=cur[:], in1=Gs[i][:], op=ALU.subtract),
                          reads=[cur_r, Gs_r[i]], writes=[R_r])
                    cur, cur_r = R, R_r
            for h in range(NH):
                t, j = h // 4, h % 4
                for i in range(3):
                    fw.dma(fw.sp, kaug[t][32 * j + 3 + i:32 * j + 4 + i, :], Gs[i][h:h + 1, :], reads=[Gs_r[i]], writes=[kaug_r[t]])
                    fw.dma(fw.sp, qaug[t][32 * j + i:32 * j + i + 1, :], NGs[i][h:h + 1, :], reads=[NGs_r[i]], writes=[qaug_r[t]])
        vsrc = self.pd["va"].rearrange("(t p) c -> p t c", p=128)
        for tkb in range(16):
            fw.dma(fw.sp, vtm[:, tkb, :], vsrc[:, tkb, :], reads=[self.pd_r["va"][tkb]], writes=[vtm_r[tkb]])
        qk_ring = Ring(fw, "qkT", [128, 2, 2, S], BF16, 2)
        pt_ring = Ring(fw, "PT", [128, 512], BF16, 4)
        rl_ring = Ring(fw, "rl", [128, 512], F32, 2)
        yo_ring = Ring(fw, "yo", [128, 512], BF16, 2)
        cnt = 0
        mg = self.mod_gen(l + 1, 5) if (l + 1 < L and self.defer_mod) else None
        for hp in range(4):
            qkt, qk_r = qk_ring.next()
            qT = qkt[:, 0]
            kT = qkt[:, 1]
            qT_r = [[qk_r] * NTB for hh in range(2)]
            kT_r = [[qk_r] * NTB for hh in range(2)]
            for hh in range(2):
                hq = hp * 2 + hh
                fw.dma(fw.sp, qkt[:, 0, hh, :], self.pd["qa"][hq * 128:(hq + 1) * 128, :], reads=self.pd_r["qa"][hq], writes=[qk_r])
                fw.dma(fw.sp, qkt[:, 1, hh, :], self.pd["ka"][hq * 128:(hq + 1) * 128, :], reads=self.pd_r["ka"][hq], writes=[qk_r])
            for hh in range(2):
                h = hp * 2 + hh
                t, j = h // 4, h % 4
                tp = (32 * j, 0)
                for qb in range(4):
                    Ob, Ob_r = self.banks[2 + cnt % 2], self.bank_r[2 + cnt % 2]
                    Lb, Lb_r = self.banks[4], self.bank_r[4]
                    cnt += 1
                    nkb = 4 * qb + 4
                    for _ in range(3):
                        if mg is not None:
                            try:
                                next(mg)
                            except StopIteration:
                                mg = None
                    def emit_qk(kb):
                        i = kb - 4 * qb
                        c0 = 128 * i if i >= 0 else 0
                        Sb, Sb_r = self.banks[SBK[kb % 4]], self.bank_r[SBK[kb % 4]]
                        qs = slice(qb * 512 + c0, (qb + 1) * 512)
                        ks = slice(kb * 128, (kb + 1) * 128)
                        fns = [lambda: nc.tensor.matmul(Sb[:, c0:512], lhsT=kT[:, hh, ks], rhs=qT[:, hh, qs], start=True, stop=False),
                               lambda: nc.tensor.matmul(Sb[:, c0:512], lhsT=kaug[t][32 * j:32 * j + 6, ks], rhs=qaug[t][32 * j:32 * j + 6, qs],
                                                        start=False, stop=True, tile_position=tp)]
                        fw.group(fw.pe, fns, reads=[kT_r[hh][kb // 4], qT_r[hh][qb], kaug_r[t], qaug_r[t]], writes=[Sb_r])
                    emit_qk(0)
                    emit_qk(1)
                    for kb in range(nkb):
                        i = kb - 4 * qb
                        c0 = 128 * i if i >= 0 else 0
                        Sb, Sb_r = self.banks[SBK[kb % 4]], self.bank_r[SBK[kb % 4]]
                        if kb + 2 < nkb:
                            emit_qk(kb + 2)
                        pt, pt_r = pt_ring.next()
                        fw.op(fw.act, lambda: nc.scalar.activation(out=pt[:, c0:512], in_=Sb[:, c0:512], func=AF.Exp),
                              reads=[Sb_r], writes=[pt_r])
                        if i >= 0:
                            fw.op(fw.dve, lambda: nc.vector.tensor_tensor(out=pt[:, c0:c0 + 128], in0=pt[:, c0:c0 + 128], in1=tri[:], op=ALU.mult),
                                  reads=[pt_r, tri_r], writes=[pt_r])
                        fns = [lambda: nc.tensor.matmul(Ob[:, c0:512], lhsT=vtm[:, kb, h * 128:(h + 1) * 128], rhs=pt[:, c0:512],
                                                        start=(kb == 0), stop=(kb == nkb - 1)),
                               lambda: nc.tensor.matmul(Lb[:, c0:512], lhsT=self.ones_bf[:], rhs=pt[:, c0:512],
                                                        start=(kb == 0), stop=(kb == nkb - 1))]
                        fw.group(fw.pe, fns, reads=[vtm_r[kb], pt_r, self.ones_r], writes=[Ob_r, Lb_r])
                    rl, rl_r = rl_ring.next()
                    fw.op(fw.dve, lambda: nc.vector.reciprocal(out=rl[:], in_=Lb[:]), reads=[Lb_r], writes=[rl_r])
                    yo, yo_r = yo_ring.next()
                    fw.op(fw.dve, lambda: nc.vector.tensor_tensor(out=yo[:], in0=Ob[:], in1=rl[:], op=ALU.mult),
                          reads=[Ob_r, rl_r], writes=[yo_r])
                    fw.dma(fw.sp, self.yf_d[h * 128:(h + 1) * 128, qb * 512:(qb + 1) * 512], yo[:], reads=[yo_r],
                           writes=[self.y_regs["yf"][h][qb]])
        if mg is not None:
            for _ in mg:
                pass

    def ret(self, l):
        nc, fw = self.nc, self.fw
        vtm = fw.sb("vtm", [128, 16, W], BF16)
        vtm_r = [Reg(f"vtm{t}") for t in range(16)]
        rotC = fw.sb("rotC", [128, S], F32)
        rotS = fw.sb("rotS", [128, S], F32)
        rot_r = Reg("rot")
        gnw = fw.sb("gnw", [128, 8], F32)
        gnw_r = Reg("gnw")
        fw.dma(fw.sp, rotC[:], self.c_rotC[:, :], writes=[rot_r])
        fw.dma(fw.sp, rotS[:], self.c_rotS[:, :], writes=[rot_r])
        fw.dma(fw.sp, gnw[:], self.gn_wT[l][:, :], writes=[gnw_r])
        vsrc = self.pd["vr"].rearrange("(t p) c -> p t c", p=128)
        for tkb in range(16):
            fw.dma(fw.sp, vtm[:, tkb, :], vsrc[:, tkb, :], reads=[self.pd_r["vr"][tkb]], writes=[vtm_r[tkb]])
        raw_ring = {n: Ring(fw, f"{n}raw", [128, S], F32, 2) for n in "qk"}
        sg_ring2 = Ring(fw, "sg", [128, S], F32, 2)
        sw = {n: fw.sb(f"{n}sw", [128, S], F32) for n in "qk"}
        sw_r = {n: Reg(f"{n}sw") for n in "qk"}
        rT = {n: fw.sb(f"{n}T", [128, S], BF16) for n in "qk"}
        rT_r = {n: Reg(f"{n}T") for n in "qk"}
        tab_ring = Ring(fw, "rtab", [128, 2, 512], F32, 1)
        at_ring = Ring(fw, "AT", [128, 512], BF16, 4)
        osb_ring = Ring(fw, "osb", [128, 512], F32, 1)
        obf_ring = Ring(fw, "obf", [128, 512], BF16, 2)
        sqb_ring = Ring(fw, "sqb", [128, 512], BF16, 2)
        m_ring = Ring(fw, "gm", [128, 512], F32, 1)
        v_ring = Ring(fw, "gv", [128, 512], F32, 1)
        yo_ring = Ring(fw, "yo", [128, 512], BF16, 2)
        cnt = 0
        pend = [None]

        def step_pend():
            if pend[0] is not None:
                try:
                    next(pend[0])
                except StopIteration:
                    pend[0] = None
        for h in range(NH):
            gamma = 1.0 - 2.0 ** (-5.0 - h)
            tab, tab_r = tab_ring.next()
            fw.dma(fw.sp, tab[:, 0, :], self.c_gfull[h], writes=[tab_r])
            fw.dma(fw.sp, tab[:, 1, :], self.c_ttri[h], writes=[tab_r])
            raw = {}
            raw_r = {}
            for n in "qk":
                raw[n], rr = raw_ring[n].next()
                raw_r[n] = [rr]
                fw.dma(fw.sp, raw[n][:], self.pd[n + "r"][h * 128:(h + 1) * 128, :], reads=self.pd_r[n + "r"][h], writes=[rr])
            sg, sgr = sg_ring2.next()
            sg_r = [sgr] * NTB
            fw.dma(fw.sp, sg[:], self.pd["gr"][h * 128:(h + 1) * 128, :], reads=self.pd_r["gr"][h], writes=[sgr])
            for n in "qk":
                fw.dma(fw.sp, sw[n][0:64, :], raw[n][64:128, :], reads=raw_r[n], writes=[sw_r[n]])
                fw.dma(fw.sp, sw[n][64:128, :], raw[n][0:64, :], reads=raw_r[n], writes=[sw_r[n]])
                RE, RH = (fw.pool, nc.gpsimd) if self.rot_on_pool else (fw.dve, nc.vector)
                fw.op(RE, lambda: RH.tensor_tensor(out=raw[n][:], in0=raw[n][:], in1=rotC[:], op=ALU.mult),
                      reads=raw_r[n] + [rot_r, sw_r[n]], writes=raw_r[n])
                fw.op(RE, lambda: RH.tensor_tensor(out=sw[n][:], in0=sw[n][:], in1=rotS[:], op=ALU.mult),
                      reads=[sw_r[n], rot_r], writes=[sw_r[n]])
                fw.op(RE, lambda: RH.tensor_tensor(out=rT[n][:], in0=raw[n][:], in1=sw[n][:], op=ALU.add),
                      reads=raw_r[n] + [sw_r[n]], writes=[rT_r[n]])

            for qb in range(4):
                Ob, Ob_r = self.banks[2 + cnt % 2], self.bank_r[2 + cnt % 2]
                Mb, Mb_r = self.banks[4], self.bank_r[4]
                Qb, Qb_r = self.banks[5], self.bank_r[5]
                cnt += 1
                nkb = 4 * qb + 4
                def emit_qk(kb):
                    i = kb - 4 * qb
                    c0 = 128 * i if i >= 0 else 0
                    Sb, Sb_r = self.banks[SBK[kb % 4]], self.bank_r[SBK[kb % 4]]
                    qs = slice(qb * 512 + c0, (qb + 1) * 512)
                    ks = slice(kb * 128, (kb + 1) * 128)
                    fw.group(fw.pe, [lambda: nc.tensor.matmul(Sb[:, c0:512], lhsT=rT["k"][:, ks], rhs=rT["q"][:, qs], start=True, stop=True)],
                             reads=[rT_r["k"], rT_r["q"]], writes=[Sb_r])
                emit_qk(0)
                emit_qk(1)
                for kb in range(nkb):
                    i = kb - 4 * qb
                    c0 = 128 * i if i >= 0 else 0
                    Sb, Sb_r = self.banks[SBK[kb % 4]], self.bank_r[SBK[kb % 4]]
                    if kb + 2 < nkb:
                        emit_qk(kb + 2)
                    at, at_r = at_ring.next()
                    if i >= 0:
                        cc = QS
                        tb_ap = tab[:, 1, 0:512 - c0]
                    else:
                        cc = QS * gamma ** (qb * 512 - kb * 128 - 128)
                        tb_ap = tab[:, 0, :]
                    fw.op(fw.dve, lambda: nc.vector.scalar_tensor_tensor(out=at[:, c0:512], in0=Sb[:, c0:512], scalar=float(cc), in1=tb_ap,
                                                                       op0=ALU.mult, op1=ALU.mult),
                          reads=[Sb_r, tab_r], writes=[at_r])
                    fw.group(fw.pe, [lambda: nc.tensor.matmul(Ob[:, c0:512], lhsT=vtm[:, kb, h * 128:(h + 1) * 128], rhs=at[:, c0:512],
                                                              start=(kb == 0), stop=(kb == nkb - 1))],
                             reads=[vtm_r[kb], at_r], writes=[Ob_r])
                    step_pend()
                def gn_gen(h=h, qb=qb, Ob=Ob, Ob_r=Ob_r, sg=sg, sg_r=sg_r):
                    osb, osb_r = osb_ring.next()
                    obf, obf_r = obf_ring.next()
                    sqb, sqb_r = sqb_ring.next()
                    fw.op(fw.act, lambda: nc.scalar.activation(out=osb[:], in_=Ob[:], func=AF.Copy), reads=[Ob_r], writes=[osb_r])
                    fw.op(fw.act, lambda: nc.scalar.activation(out=obf[:], in_=Ob[:], func=AF.Copy), reads=[Ob_r], writes=[obf_r])
                    fw.op(fw.act, lambda: nc.scalar.activation(out=sqb[:], in_=Ob[:], func=AF.Square), reads=[Ob_r], writes=[sqb_r])
                    fw.group(fw.pe, [lambda: nc.tensor.matmul(Mb[:], lhsT=self.ones_bf[:], rhs=obf[:], start=True, stop=True)],
                             reads=[obf_r, self.ones_r], writes=[Mb_r])
                    fw.group(fw.pe, [lambda: nc.tensor.matmul(Qb[:], lhsT=self.ones_bf[:], rhs=sqb[:], start=True, stop=True)],
                             reads=[sqb_r, self.ones_r], writes=[Qb_r])
                    yield
                    gm, gm_r = m_ring.next()
                    gv, gv_r = v_ring.next()
                    fw.op(fw.dve, lambda: nc.vector.tensor_scalar(out=gm[:], in0=Mb[:], scalar1=1.0 / HD, scalar2=None, op0=ALU.mult),
                          reads=[Mb_r], writes=[gm_r])
                    yield
                    fw.op(fw.dve, lambda: nc.vector.tensor_tensor(out=gv[:], in0=gm[:], in1=gm[:], op=ALU.mult), reads=[gm_r], writes=[gv_r])
                    yield
                    fw.op(fw.dve, lambda: nc.vector.scalar_tensor_tensor(out=gv[:], in0=Qb[:], scalar=1.0 / HD, in1=gv[:], op0=ALU.mult, op1=ALU.subtract),
                          reads=[Qb_r, gv_r], writes=[gv_r])
                    fw.op(fw.act, lambda: nc.scalar.activation(out=gv[:], in_=gv[:], func=AF.Sqrt, bias=self.eps_t[:], scale=1.0),
                          reads=[gv_r, self.eps_r], writes=[gv_r])
                    yield
                    fw.op(fw.dve, lambda: nc.vector.tensor_tensor(out=osb[:], in0=osb[:], in1=gm[:], op=ALU.subtract), reads=[osb_r, gm_r], writes=[osb_r])
                    yield
                    fw.op(fw.dve, lambda: nc.vector.reciprocal(out=gv[:], in_=gv[:]), reads=[gv_r], writes=[gv_r])
                    yield
                    fw.op(fw.dve, lambda: nc.vector.tensor_tensor(out=osb[:], in0=osb[:], in1=gv[:], op=ALU.mult), reads=[osb_r, gv_r], writes=[osb_r])
                    yield
                    yo, yo_r = yo_ring.next()
                    fw.op(fw.dve, lambda: nc.vector.scalar_tensor_tensor(out=yo[:], in0=osb[:], scalar=gnw[:, h:h + 1], in1=sg[:, qb * 512:(qb + 1) * 512],
                                                                       op0=ALU.mult, op1=ALU.mult),
                          reads=[osb_r, gnw_r, sg_r[qb]], writes=[yo_r])
                    fw.dma(fw.sp, self.yr_d[h * 128:(h + 1) * 128, qb * 512:(qb + 1) * 512], yo[:], reads=[yo_r],
                           writes=[self.y_regs["yr"][h][qb]])
                if self.gn_lazy:
                    if pend[0] is not None:
                        for _ in pend[0]:
                            pass
                    pend[0] = gn_gen()
                else:
                    for _ in gn_gen():
                        pass
        if pend[0] is not None:
            for _ in pend[0]:
                pass

    def merge(self, l):
        nc, fw = self.nc, self.fw
        o = (l * 3 + 1) * KC
        g_ap = self.gvec[:, o:o + KC]
        TP = 2 * TB
        wg_ring = Ring(fw, "wg", [128, KC, 128], BF16, 4)
        wb_ring = Ring(fw, "wb", [128, 8, 128], BF16, 4)
        bg = fw.sb("bg", [128, 3 * KC], F32)
        bg_r = Reg("bg")
        fw.dma(fw.sp, bg[:], self.b_gateT[l][:, :], writes=[bg_r])
        yb = [fw.sb(f"yb{br}", [128, 8, TP], BF16) for br in range(3)]
        yb_r = [Reg(f"yb{br}") for br in range(3)]
        mT = fw.sb("mT", [128, KC, TP], BF16)
        mT_r = [[Reg(f"mT{dc}_{t2}") for t2 in range(2)] for dc in range(KC)]
        sg_ring = Ring(fw, "msg", [128, TB], F32, 4)
        acc_ring = Ring(fw, "macc", [128, TB], F32, 4)
        t_ring = Ring(fw, "mt", [128, TB], F32, 2)
        xo_ring = Ring(fw, "xo", [128, TB], F32, 4)
        ysrc = [d.rearrange("(c p) t -> p c t", p=128) for d in (self.yf_d, self.ys_d, self.yr_d)]
        ynames = ["yf", "ys", "yr"]
        wbr = self.w_br[l].rearrange("(c p) d -> p c d", p=128)
        wo = self.w_out[l].rearrange("(kc p) d -> p kc d", p=128)
        gcnt = 0
        bcnt = 0
        ocnt = 0
        for tp in range(S // TP):
            for br in range(3):
                fw.dma(fw.sp, yb[br][:], ysrc[br][:, :, tp * TP:(tp + 1) * TP],
                       reads=[self.y_regs[ynames[br]][c][2 * tp + t2] for c in range(8) for t2 in range(2)], writes=[yb_r[br]])
            for dc in range(KC):
                accs = [acc_ring.next() for _ in range(2)]
                for br in range(3):
                    wg, wg_r = wg_ring.next()
                    fw.dma(fw.pool, wg[:], self.win_cols(l, OFF["gl"] + br * D + dc * 128, 128), writes=[wg_r])
                    wb, wb_r = wb_ring.next()
                    fw.dma(fw.pool, wb[:], wbr[:, br * 8:(br + 1) * 8, dc * 128:(dc + 1) * 128], writes=[wb_r])
                    for t2 in range(2):
                        tb = 2 * tp + t2
                        ts = slice(tb * TB, (tb + 1) * TB)
                        t2s = slice(t2 * TB, (t2 + 1) * TB)
                        gb, gb_r = self.banks[gcnt % 3], self.bank_r[gcnt % 3]
                        gcnt += 1
                        fns = [lambda kc=kc: nc.tensor.matmul(gb[:], lhsT=wg[:, kc, :], rhs=self.hT[:, kc, ts], start=(kc == 0), stop=(kc == KC - 1))
                               for kc in range(KC)]
                        fw.group(fw.pe, fns, reads=[wg_r] + self.h_regs(tb), writes=[gb_r])
                        sgt, sgt_r = sg_ring.next()
                        fw.op(fw.act, lambda: nc.scalar.activation(out=sgt[:], in_=gb[:], func=AF.Sigmoid, bias=bg[:, br * KC + dc:br * KC + dc + 1], scale=1.0),
                              reads=[gb_r, bg_r], writes=[sgt_r])
                        bb, bb_r = self.banks[3 + bcnt % 3], self.bank_r[3 + bcnt % 3]
                        bcnt += 1
                        fns = [lambda kc=kc: nc.tensor.matmul(bb[:], lhsT=wb[:, kc, :], rhs=yb[br][:, kc, t2s], start=(kc == 0), stop=(kc == 7))
                               for kc in range(8)]
                        fw.group(fw.pe, fns, reads=[wb_r, yb_r[br]], writes=[bb_r])
                        acc, acc_r = accs[t2]
                        if br == 0:
                            fw.op(fw.dve, lambda: nc.vector.tensor_tensor(out=acc[:], in0=bb[:], in1=sgt[:], op=ALU.mult),
                                  reads=[bb_r, sgt_r], writes=[acc_r])
                        else:
                            tt, tt_r = t_ring.next()
                            fw.op(fw.dve, lambda: nc.vector.tensor_tensor(out=tt[:], in0=bb[:], in1=sgt[:], op=ALU.mult),
                                  reads=[bb_r, sgt_r], writes=[tt_r])
                            if br == 1:
                                fw.op(fw.dve, lambda: nc.vector.tensor_tensor(out=acc[:], in0=acc[:], in1=tt[:], op=ALU.add),
                                      reads=[acc_r, tt_r], writes=[acc_r])
                            else:
                                fw.op(fw.dve, lambda: nc.vector.tensor_tensor(out=mT[:, dc, t2s], in0=acc[:], in1=tt[:], op=ALU.add),
                                      reads=[acc_r, tt_r], writes=[mT_r[dc][t2]])
            xsrc = self.x_cur
            for dc in range(KC):
                wg, wg_r = wg_ring.next()
                fw.dma(fw.pool, wg[:], wo[:, :, dc * 128:(dc + 1) * 128], writes=[wg_r])
                for t2 in range(2):
                    tb = 2 * tp + t2
                    ts = slice(tb * TB, (tb + 1) * TB)
                    t2s = slice(t2 * TB, (t2 + 1) * TB)
                    ob, ob_r = self.banks[6 + ocnt % 2], self.bank_r[6 + ocnt % 2]
                    ocnt += 1
                    xo, xo_r = xo_ring.next()
                    fw.dma(fw.act, xo[:], xsrc[dc * 128:(dc + 1) * 128, ts], reads=[self.x_regs[dc][tb]], writes=[xo_r])
                    fns = [lambda kc=kc: nc.tensor.matmul(ob[:], lhsT=wg[:, kc, :], rhs=mT[:, kc, t2s], start=(kc == 0), stop=(kc == KC - 1))
                           for kc in range(KC)]
                    fw.group(fw.pe, fns, reads=[wg_r] + [mT_r[kc][t2] for kc in range(KC)], writes=[ob_r])
                    fw.op(fw.dve, lambda: nc.vector.scalar_tensor_tensor(out=xo[:], in0=ob[:], scalar=g_ap[:, dc:dc + 1], in1=xo[:], op0=ALU.mult, op1=ALU.add),
                          reads=[ob_r, xo_r, self.gv_r], writes=[xo_r])
                    fw.dma(fw.sp, self.xres[dc * 128:(dc + 1) * 128, ts], xo[:], reads=[xo_r], writes=[self.x_regs[dc][tb]])
        self.x_cur = self.xres

    def cs16(self, th, N, c, s, tmp):
        nc, fw = self.nc, self.fw
        r = self._ssm_r
        t1, t2, t3 = tmp
        fw.op(fw.dve, lambda: nc.vector.tensor_scalar(out=t1[:], in0=th[:], scalar1=1.0 / 16, scalar2=None, op0=ALU.mult), reads=[r], writes=[r])
        fw.op(fw.act, lambda: nc.scalar.activation(out=s[:], in_=t1[:], func=AF.Sin), reads=[r], writes=[r])
        fw.op(fw.act, lambda: nc.scalar.activation(out=c[:], in_=t1[:], func=AF.Sin, bias=self.halfpi[:], scale=1.0), reads=[r], writes=[r])
        for _ in range(4):
            self.csq(c, s, tmp)

    def csq(self, c, s, tmp):
        nc, fw = self.nc, self.fw
        r = self._ssm_r
        t1, t2, t3 = tmp
        fw.op(fw.dve, lambda: nc.vector.tensor_tensor(out=t1[:], in0=c[:], in1=c[:], op=ALU.mult), reads=[r], writes=[r])
        fw.op(fw.dve, lambda: nc.vector.tensor_tensor(out=t2[:], in0=s[:], in1=s[:], op=ALU.mult), reads=[r], writes=[r])
        fw.op(fw.dve, lambda: nc.vector.tensor_tensor(out=t3[:], in0=c[:], in1=s[:], op=ALU.mult), reads=[r], writes=[r])
        fw.op(fw.dve, lambda: nc.vector.tensor_tensor(out=c[:], in0=t1[:], in1=t2[:], op=ALU.subtract), reads=[r], writes=[r])
        fw.op(fw.dve, lambda: nc.vector.tensor_scalar(out=s[:], in0=t3[:], scalar1=2.0, scalar2=None, op0=ALU.mult), reads=[r], writes=[r])

    def ssm_tiles(self):
        fw = self.fw
        rS = fw.sb("rS", [128, 32], F32)
        cosT = fw.sb("cosT", [128, 32, 128], F32)
        sinT = fw.sb("sinT", [128, 32, 128], F32)
        BD = [fw.sb(f"BD{i}", [128, 8, 4, 128], BF16) for i in range(2)]
        CBD = [fw.sb(f"CBD{i}", [128, 32, 32], BF16) for i in range(2)]
        return rS, cosT, sinT, BD, CBD

    def ssm_tab_pairs(self, l, tiles):
        rS, cosT, sinT, BD, CBD = tiles
        d = self.sst_d[l]
        return [(rS[:], d["rS"][:, :]), (cosT[:].rearrange("p j t -> p (j t)"), d["cosT"][:, :]),
                (sinT[:].rearrange("p j t -> p (j t)"), d["sinT"][:, :]),
                (BD[0][:].rearrange("p k j q -> p (k j q)"), d["BD0"][:, :]), (BD[1][:].rearrange("p k j q -> p (k j q)"), d["BD1"][:, :]),
                (CBD[0][:].rearrange("p j n -> p (j n)"), d["CBD0"][:, :]), (CBD[1][:].rearrange("p j n -> p (j n)"), d["CBD1"][:, :])]

    def ssm_precompute(self, l):
        fw = self.fw
        self._ssm_r = Reg(f"ssm_setup{l}")
        tiles = self.ssm_tiles()
        self.ssm_setup(l, *tiles)
        for sb_ap, d_ap in self.ssm_tab_pairs(l, tiles):
            fw.dma(fw.sp, d_ap, sb_ap, reads=[self._ssm_r], writes=[self.sst_r[l]])
        self.sst_done.add(l)

    def ssm(self, l):
        nc, fw = self.nc, self.fw
        V = nc.vector
        self._ssm_r = Reg("ssm_main")
        r = self._ssm_r
        tiles = self.ssm_tiles()
        rS, cosT, sinT, BD, CBD = tiles
        if l in self.sst_done:
            for sb_ap, d_ap in self.ssm_tab_pairs(l, tiles):
                fw.dma(fw.sp, sb_ap, d_ap, reads=[self.sst_r[l]], writes=[r])
        else:
            with fw.scope():
                self.ssm_setup(l, *tiles)
        DT = fw.sb("DT", [128, 8], F32)
        glub = fw.sb("glub", [128, 8], F32)
        fw.dma(fw.sp, DT[:], self.sD_T[l][:, :], writes=[r])
        fw.dma(fw.sp, glub[:], self.glu_bT[l][:, :], writes=[r])
        self.ssm_main(l, rS, cosT, sinT, BD, CBD, DT, glub, r)

    def ssm_setup(self, l, rS, cosT, sinT, BD, CBD):
        nc, fw = self.nc, self.fw
        V = nc.vector
        r = self._ssm_r
        self.halfpi = fw.sb("halfpi", [128, 1], F32)
        fw.op(fw.dve, lambda: V.memset(self.halfpi[:], math.pi / 2), writes=[r])

        def dve(fn):
            fw.op(fw.dve, fn, reads=[r], writes=[r])

        def act(fn):
            fw.op(fw.act, fn, reads=[r], writes=[r])

        if True:
            mrow = fw.sb("mrow", [128, 8], F32)
            mst = fw.sb("mst", [128, 2], F32)
            fw.dma(fw.sp, mrow[:], self.c_mrow[:, :], writes=[r])
            fw.dma(fw.sp, mst[:], self.c_mst[:, :], writes=[r])
            are = fw.sb("are", [128, 32], F32)
            aim = fw.sb("aim", [128, 32], F32)
            ldt = fw.sb("ldt", [128, 32], F32)
            fw.dma(fw.sp, are[:], self.sA_reS[l][:, :], writes=[r])
            fw.dma(fw.sp, aim[:], self.sA_imS[l][:, :], writes=[r])
            fw.dma(fw.sp, ldt[:], self.sldtS[l][:, :], writes=[r])
            dt = fw.sb("dt", [128, 32], F32)
            th = fw.sb("th", [128, 32], F32)
            c1 = fw.sb("c1", [128, 32], F32)
            s1 = fw.sb("s1", [128, 32], F32)
            tmpS = [fw.sb(f"tmpS{i}", [128, 32], F32) for i in range(3)]
            act(lambda: nc.scalar.activation(out=dt[:], in_=ldt[:], func=AF.Exp))
            dve(lambda: V.tensor_tensor(out=th[:], in0=are[:], in1=dt[:], op=ALU.mult))
            act(lambda: nc.scalar.activation(out=rS[:], in_=th[:], func=AF.Exp))
            dve(lambda: V.tensor_tensor(out=th[:], in0=aim[:], in1=dt[:], op=ALU.mult))
            self.cs16(th, 32, c1, s1, tmpS)
            tA = fw.sb("tA", [128, 32, 64], F32)
            tB = fw.sb("tB", [128, 32, 64], F32)
            dve(lambda: V.tensor_copy(out=cosT[:, :, 0:1], in_=c1[:, :].unsqueeze(2)))
            dve(lambda: V.tensor_copy(out=sinT[:, :, 0:1], in_=s1[:, :].unsqueeze(2)))
            n = 1
            while n < 128:
                pc = c1[:, :].unsqueeze(2).to_broadcast([128, 32, n])
                ps_ = s1[:, :].unsqueeze(2).to_broadcast([128, 32, n])
                dve(lambda: V.tensor_tensor(out=tA[:, :, 0:n], in0=cosT[:, :, 0:n], in1=pc, op=ALU.mult))
                dve(lambda: V.tensor_tensor(out=tB[:, :, 0:n], in0=sinT[:, :, 0:n], in1=ps_, op=ALU.mult))
                dve(lambda: V.tensor_tensor(out=cosT[:, :, n:2 * n], in0=tA[:, :, 0:n], in1=tB[:, :, 0:n], op=ALU.subtract))
                dve(lambda: V.tensor_tensor(out=tA[:, :, 0:n], in0=cosT[:, :, 0:n], in1=ps_, op=ALU.mult))
                dve(lambda: V.tensor_tensor(out=tB[:, :, 0:n], in0=sinT[:, :, 0:n], in1=pc, op=ALU.mult))
                dve(lambda: V.tensor_tensor(out=sinT[:, :, n:2 * n], in0=tA[:, :, 0:n], in1=tB[:, :, 0:n], op=ALU.add))
                n *= 2
                if n < 128:
                    self.csq(c1, s1, tmpS)
            Cre = fw.sb("Cre", [128, 32, 16], F32)
            Cim = fw.sb("Cim", [128, 32, 16], F32)
            fw.dma(fw.sp, Cre[:], self.sC_reS[l].rearrange("p (j n) -> p j n", n=16), writes=[r])
            fw.dma(fw.sp, Cim[:], self.sC_imS[l].rearrange("p (j n) -> p j n", n=16), writes=[r])
            for g2 in range(2):
                dve(lambda: V.tensor_scalar(out=CBD[0][:, :, g2 * 16:(g2 + 1) * 16], in0=Cre[:], scalar1=mst[:, g2:g2 + 1], scalar2=None, op0=ALU.mult))
                dve(lambda: V.tensor_scalar(out=CBD[1][:, :, g2 * 16:(g2 + 1) * 16], in0=Cim[:], scalar1=mst[:, g2:g2 + 1], scalar2=-1.0,
                                            op0=ALU.mult, op1=ALU.mult))
            R = {}
            for nm, src in (("are", self.sA_reR), ("aim", self.sA_imR), ("ldt", self.sldtR), ("Bre", self.sB_reR), ("Bim", self.sB_imR)):
                R[nm] = fw.sb("R" + nm, [128, 512], F32)
                fw.dma(fw.sp, R[nm][:], src[l][:, :], writes=[r])
            for nm in ("dt", "th", "mag", "c", "s", "abre", "abim", "den", "t1", "t2", "t3", "cre", "cim"):
                R[nm] = fw.sb("R" + nm, [128, 512], F32)
            act(lambda: nc.scalar.activation(out=R["dt"][:], in_=R["ldt"][:], func=AF.Exp))
            dve(lambda: V.tensor_tensor(out=R["th"][:], in0=R["are"][:], in1=R["dt"][:], op=ALU.mult))
            act(lambda: nc.scalar.activation(out=R["mag"][:], in_=R["th"][:], func=AF.Exp))
            dve(lambda: V.tensor_tensor(out=R["th"][:], in0=R["aim"][:], in1=R["dt"][:], op=ALU.mult))
            self.cs16(R["th"], 512, R["c"], R["s"], [R["t1"], R["t2"], R["t3"]])
            dve(lambda: V.tensor_tensor(out=R["abre"][:], in0=R["mag"][:], in1=R["c"][:], op=ALU.mult))
            dve(lambda: V.tensor_tensor(out=R["abim"][:], in0=R["mag"][:], in1=R["s"][:], op=ALU.mult))
            dve(lambda: V.tensor_scalar(out=R["abre"][:], in0=R["abre"][:], scalar1=-1.0, scalar2=None, op0=ALU.add))
            dve(lambda: V.tensor_tensor(out=R["t1"][:], in0=R["are"][:], in1=R["are"][:], op=ALU.mult))
            dve(lambda: V.tensor_tensor(out=R["t2"][:], in0=R["aim"][:], in1=R["aim"][:], op=ALU.mult))
            dve(lambda: V.tensor_tensor(out=R["den"][:], in0=R["t1"][:], in1=R["t2"][:], op=ALU.add))
            dve(lambda: V.reciprocal(out=R["den"][:], in_=R["den"][:]))
            dve(lambda: V.tensor_tensor(out=R["t1"][:], in0=R["abre"][:], in1=R["are"][:], op=ALU.mult))
            dve(lambda: V.tensor_tensor(out=R["t2"][:], in0=R["abim"][:], in1=R["aim"][:], op=ALU.mult))
            dve(lambda: V.tensor_tensor(out=R["t1"][:], in0=R["t1"][:], in1=R["t2"][:], op=ALU.add))
            dve(lambda: V.tensor_tensor(out=R["cre"][:], in0=R["t1"][:], in1=R["den"][:], op=ALU.mult))
            dve(lambda: V.tensor_tensor(out=R["t1"][:], in0=R["abim"][:], in1=R["are"][:], op=ALU.mult))
            dve(lambda: V.tensor_tensor(out=R["t2"][:], in0=R["abre"][:], in1=R["aim"][:], op=ALU.mult))
            dve(lambda: V.tensor_tensor(out=R["t1"][:], in0=R["t1"][:], in1=R["t2"][:], op=ALU.subtract))
            dve(lambda: V.tensor_tensor(out=R["cim"][:], in0=R["t1"][:], in1=R["den"][:], op=ALU.mult))
            dve(lambda: V.tensor_tensor(out=R["t1"][:], in0=R["cre"][:], in1=R["Bre"][:], op=ALU.mult))
            dve(lambda: V.tensor_tensor(out=R["t2"][:], in0=R["cim"][:], in1=R["Bim"][:], op=ALU.mult))
            dve(lambda: V.tensor_tensor(out=R["t3"][:], in0=R["t1"][:], in1=R["t2"][:], op=ALU.subtract))
            dve(lambda: V.tensor_tensor(out=R["t1"][:], in0=R["cre"][:], in1=R["Bim"][:], op=ALU.mult))
            dve(lambda: V.tensor_tensor(out=R["t2"][:], in0=R["cim"][:], in1=R["Bre"][:], op=ALU.mult))
            dve(lambda: V.tensor_tensor(out=R["t1"][:], in0=R["t1"][:], in1=R["t2"][:], op=ALU.add))
            for jj in range(4):
                for g2 in range(2):
                    mc = mrow[:, jj * 2 + g2:jj * 2 + g2 + 1]
                    dve(lambda: V.tensor_scalar(out=BD[0][:, :, jj, g2 * 64:(g2 + 1) * 64], in0=R["t3"][:].rearrange("p (k q) -> p k q", q=64),
                                                scalar1=mc, scalar2=None, op0=ALU.mult))
                    dve(lambda: V.tensor_scalar(out=BD[1][:, :, jj, g2 * 64:(g2 + 1) * 64], in0=R["t1"][:].rearrange("p (k q) -> p k q", q=64),
                                                scalar1=mc, scalar2=None, op0=ALU.mult))

    def ssm_main(self, l, rS, cosT, sinT, BD, CBD, DT, glub, r):
        nc, fw = self.nc, self.fw
        V = nc.vector
        usT = fw.sb("usT", [128, 8, S], BF16)
        usT_r = [[Reg(f"usT{c}_{tb}") for tb in range(NTB)] for c in range(8)]

        def cons_us(bk, bk_r, ci, tb, m):
            fw.op(fw.act, lambda: nc.scalar.activation(out=usT[:, ci, tb * TB:(tb + 1) * TB], in_=bk[:], func=AF.Copy),
                  reads=[bk_r], writes=[usT_r[ci][tb]])
        with fw.scope():
            wring = Ring(fw, "wA", [128, KC, 256], BF16, 2)
            self.projA(self.win_cols(l, OFF["us"], W), W, KC, self.h_rhs, self.h_regs, cons_us, [6, 7], wring)
        with fw.scope():
            self.ssm_scan(l, usT, usT_r, rS, cosT, sinT, BD, CBD, DT, r)
        with fw.scope():
            self.ssm_glu(l, glub, r)

    def ssm_scan(self, l, usT, usT_r, rS, cosT, sinT, BD, CBD, DT, r):
        nc, fw = self.nc, self.fw
        V = nc.vector
        pg = self.proj_items(l)
        go_ring = Ring(fw, "gout", [128, 512], BF16, 2)
        tmp_ring = Ring(fw, "st", [128, 512], F32, 4)
        b_ring = Ring(fw, "sb", [128, 2, 512], F32, 1)
        b_regs = (Reg("b_re"), Reg("b_im"))
        xb_ring = Ring(fw, "sxb", [128, 2, 512], BF16, 2)
        carry = [fw.sb(f"carry{i}", [128, 2, 4], F32) for i in range(2)]
        carry_r = [Reg(f"carry{i}") for i in range(2)]
        yp_ring = Ring(fw, "yp", [128, 512], F32, 1)
        g1_ring = Ring(fw, "g1", [128, 512], F32, 1)
        g2_ring = Ring(fw, "g2", [128, 512], F32, 1)
        units = [(kc, ch) for kc in range(8) for ch in range(16)]
        NU = len(units)
        st = {}

        def banksA(u):
            return (self.banks[0 + u % 2], self.bank_r[0 + u % 2], self.banks[2 + u % 2], self.bank_r[2 + u % 2])

        def emit_A(u):
            kc, ch = units[u]
            tb = ch // 4
            cs_ = slice(ch * 128, (ch + 1) * 128)
            Are, Are_r, Aim, Aim_r = banksA(u)
            for (A, A_r, bd) in ((Are, Are_r, BD[0]), (Aim, Aim_r, BD[1])):
                fns = [lambda jj=jj: nc.tensor.matmul(A[:, jj * 128:(jj + 1) * 128], lhsT=bd[:, kc, jj, :],
                                                     rhs=usT[:, kc, cs_], start=True, stop=True) for jj in range(4)]
                fw.group(fw.pe, fns, reads=[r, usT_r[kc][tb]], writes=[A_r])

        def emit_D(u):
            kc, ch = units[u]
            cosv = cosT[:, 4 * kc:4 * kc + 4, :]
            sinv = sinT[:, 4 * kc:4 * kc + 4, :]
            if ch == 0:
                fw.op(fw.dve, lambda: V.memset(carry[0][:], 0.0), writes=[carry_r[0]])
            Are, Are_r, Aim, Aim_r = banksA(u)
            Ar3 = Are[:].rearrange("p (j t) -> p j t", t=128)
            Ai3 = Aim[:].rearrange("p (j t) -> p j t", t=128)
            tt_ = [tmp_ring.next() for _ in range(4)]
            T3 = [t[:].rearrange("p (j t) -> p j t", t=128) for (t, _) in tt_]
            TR = [tr for (_, tr) in tt_]
            bt, _unused = b_ring.next()
            bt_r = b_regs
            b3 = [bt[:, i, :].rearrange("p (j t) -> p j t", t=128) for i in range(2)]
            bre_r, bim_r = bt_r
            fw.op(fw.dve, lambda: V.tensor_tensor(out=T3[0], in0=Ar3, in1=cosv, op=ALU.mult), reads=[Are_r, r], writes=[TR[0]])
            fw.op(fw.dve, lambda: V.tensor_tensor(out=T3[1], in0=Ai3, in1=sinv, op=ALU.mult), reads=[Aim_r, r], writes=[TR[1]])
            fw.op(fw.dve, lambda: V.tensor_tensor(out=T3[2], in0=Ai3, in1=cosv, op=ALU.mult), reads=[Aim_r, r], writes=[TR[2]])
            fw.op(fw.dve, lambda: V.tensor_tensor(out=T3[3], in0=Ar3, in1=sinv, op=ALU.mult), reads=[Are_r, r], writes=[TR[3]])
            fw.op(fw.dve, lambda: V.tensor_tensor(out=b3[0], in0=T3[0], in1=T3[1], op=ALU.add), reads=[TR[0], TR[1]], writes=[bre_r])
            fw.op(fw.dve, lambda: V.tensor_tensor(out=b3[1], in0=T3[2], in1=T3[3], op=ALU.subtract), reads=[TR[2], TR[3]], writes=[bim_r])
            cin, cin_r = carry[ch % 2], carry_r[ch % 2]
            cout, cout_r = carry[(ch + 1) % 2], carry_r[(ch + 1) % 2]
            xbanks = ((Are, Are_r), (Aim, Aim_r))
            for i in range(2):
                xo_, xo_r_ = xbanks[i]
                for jj in range(4):
                    j = 4 * kc + jj
                    fw.op(fw.dve, lambda: V.tensor_tensor_scan(
                        out=xo_[:, jj * 128:(jj + 1) * 128], data0=rS[:, j:j + 1].to_broadcast([128, 128]),
                        data1=bt[:, i, jj * 128:(jj + 1) * 128], initial=cin[:, i, jj:jj + 1], op0=ALU.mult, op1=ALU.add),
                        reads=[bt_r[i], cin_r, r], writes=[xo_r_])
            x3 = [Ar3, Ai3]
            xb, xb_r = xb_ring.next()
            xb3 = [xb[:, i, :].rearrange("p (j t) -> p j t", t=128) for i in range(2)]
            fw.op(fw.dve, lambda: V.tensor_tensor(out=T3[0], in0=x3[0], in1=cosv, op=ALU.mult), reads=[Are_r, r], writes=[TR[0]])
            fw.op(fw.dve, lambda: V.tensor_tensor(out=T3[2], in0=x3[0], in1=sinv, op=ALU.mult), reads=[Are_r, r], writes=[TR[2]])
            fw.op(fw.dve, lambda: V.tensor_tensor(out=T3[1], in0=x3[1], in1=sinv, op=ALU.mult), reads=[Aim_r, r], writes=[TR[1]])
            fw.op(fw.dve, lambda: V.tensor_tensor(out=T3[3], in0=x3[1], in1=cosv, op=ALU.mult), reads=[Aim_r, r], writes=[TR[3]])
            fw.op(fw.dve, lambda: V.tensor_tensor(out=xb3[0], in0=T3[0], in1=T3[1], op=ALU.subtract), reads=[TR[0], TR[1]], writes=[xb_r])
            fw.op(fw.dve, lambda: V.tensor_tensor(out=xb3[1], in0=T3[2], in1=T3[3], op=ALU.add), reads=[TR[2], TR[3]], writes=[xb_r])
            fw.op(fw.dve, lambda: V.tensor_tensor(out=cout[:, 0, :], in0=T3[0][:, :, 127], in1=T3[1][:, :, 127], op=ALU.subtract),
                  reads=[TR[0], TR[1]], writes=[cout_r])
            fw.op(fw.dve, lambda: V.tensor_tensor(out=cout[:, 1, :], in0=T3[2][:, :, 127], in1=T3[3][:, :, 127], op=ALU.add),
                  reads=[TR[2], TR[3]], writes=[cout_r])
            st[u] = (xb, xb_r)

        def emit_C(u):
            kc, ch = units[u]
            tb = ch // 4
            xb, xb_r = st.pop(u)
            Yb, Yb_r = self.banks[4 + tb % 2], self.bank_r[4 + tb % 2]
            yc = slice((ch % 4) * 128, (ch % 4 + 1) * 128)
            fns = []
            for jj in range(4):
                j = 4 * kc + jj
                fns.append(lambda jj=jj, j=j: nc.tensor.matmul(Yb[32 * jj:32 * jj + 32, yc], lhsT=CBD[0][:, j, :], rhs=xb[:, 0, jj * 128:(jj + 1) * 128],
                                                              start=True, stop=False, tile_position=(0, 32 * jj)))
                fns.append(lambda jj=jj, j=j: nc.tensor.matmul(Yb[32 * jj:32 * jj + 32, yc], lhsT=CBD[1][:, j, :], rhs=xb[:, 1, jj * 128:(jj + 1) * 128],
                                                              start=False, stop=True, tile_position=(0, 32 * jj)))
            fw.group(fw.pe, fns, reads=[xb_r, r], writes=[Yb_r])

        def emit_G(u):
            kc, ch = units[u]
            if ch % 4 != 3:
                return
            tb = ch // 4
            Yb, Yb_r = self.banks[4 + tb % 2], self.bank_r[4 + tb % 2]
            ts = slice(tb * TB, (tb + 1) * TB)
            yp, yp_r = yp_ring.next()
            fw.op(fw.dve, lambda: V.scalar_tensor_tensor(out=yp[:], in0=usT[:, kc, ts], scalar=DT[:, kc:kc + 1], in1=Yb[:], op0=ALU.mult, op1=ALU.add),
                  reads=[usT_r[kc][tb], Yb_r, r], writes=[yp_r])
            ga, ga_r = g1_ring.next()
            gb, gb_r = g2_ring.next()
            fw.op(fw.dve, lambda: V.tensor_tensor(out=ga[:], in0=yp[:], in1=yp[:], op=ALU.mult), reads=[yp_r], writes=[ga_r])
            fw.op(fw.dve, lambda: V.tensor_scalar(out=ga[:], in0=ga[:], scalar1=0.044715, scalar2=1.0, op0=ALU.mult, op1=ALU.add),
                  reads=[ga_r], writes=[ga_r])
            fw.op(fw.dve, lambda: V.tensor_tensor(out=ga[:], in0=ga[:], in1=yp[:], op=ALU.mult), reads=[ga_r, yp_r], writes=[ga_r])
            fw.op(fw.act, lambda: nc.scalar.activation(out=gb[:], in_=ga[:], func=AF.Sigmoid, scale=1.5957691216057308),
                  reads=[ga_r], writes=[gb_r])
            go, go_r = go_ring.next()
            fw.op(fw.dve, lambda: V.tensor_tensor(out=go[:], in0=yp[:], in1=gb[:], op=ALU.mult), reads=[yp_r, gb_r], writes=[go_r])
            fw.dma(fw.sp, self.pd["g"][kc * 128:(kc + 1) * 128, ts], go[:], reads=[go_r], writes=[self.pd_r["g"][kc][tb]])

        def step_pg(n):
            nonlocal pg
            for _ in range(n):
                if pg is not None:
                    try:
                        next(pg)
                    except StopIteration:
                        pg = None

        emit_A(0)
        for u in range(NU):
            emit_D(u)
            if u + 1 < NU:
                emit_A(u + 1)
            step_pg(3)
            emit_C(u)
            if u >= 1:
                emit_G(u - 1)
        emit_G(NU - 1)
        if pg is not None:
            for _ in pg:
                pass

    def ssm_glu(self, l, glub, r):
        nc, fw = self.nc, self.fw
        V = nc.vector
        gT = fw.sb("gT", [128, 8, S], BF16)
        gT_r = [[Reg(f"gT{c}_{tb}") for tb in range(NTB)] for c in range(8)]
        gsrc = self.pd["g"].rearrange("(c p) t -> p c t", p=128)
        for c in range(8):
            fw.dma(fw.sp, gT[:, c, :], gsrc[:, c, :], reads=self.pd_r["g"][c], writes=gT_r[c])
        wring = Ring(fw, "wA", [128, KC, 256], BF16, 2)
        sg_ring = Ring(fw, "gsg", [128, 512], F32, 2)
        yo_ring = Ring(fw, "yo", [128, 512], BF16, 2)

        def cons_glu(bk, bk_r, ci, tb, m):
            ts = slice(tb * TB, (tb + 1) * TB)
            sgt, sgt_r = sg_ring.next()
            fw.op(fw.act, lambda: nc.scalar.activation(out=sgt[:], in_=bk[:], func=AF.Sigmoid, bias=glub[:, ci:ci + 1], scale=1.0),
                  reads=[bk_r, r], writes=[sgt_r])
            yo, yo_r = yo_ring.next()
            fw.op(fw.dve, lambda: V.tensor_tensor(out=yo[:], in0=gT[:, ci, ts], in1=sgt[:], op=ALU.mult), reads=[gT_r[ci][tb], sgt_r], writes=[yo_r])
            fw.dma(fw.sp, self.ys_d[ci * 128:(ci + 1) * 128, ts], yo[:], reads=[yo_r], writes=[self.y_regs["ys"][ci][tb]])
        gw = self.glu_w[l].rearrange("(kc p) n -> p kc n", p=128)
        self.projA(gw, W, 8, lambda kc, ts: gT[:, kc, ts], lambda tb: [gT_r[kc][tb] for kc in range(8)], cons_glu, [6, 7], wring)

    rot_on_pool = False
    sst_in_prologue = False
    gn_lazy = True

    def build(self):
        fw = self.fw
        self.defer_mod = any(st[0] == "mix" and st[1] == 0 and (len(st) < 3 or "fox" in st[2]) for st in self.stages)
        self.sst_done = set()
        stages = list(self.stages)
        self.alloc_persistent()
        with fw.scope():
            self.prologue()
            mg = self.mod_gen(0, 7)
            for _ in range(24):
                next(mg)
            if stages and stages[0][0] == "ffn" and stages[0][1] == 0 and stages[0][2] == 0:
                self.mg_live = mg
                self.ffn(0, 0)
                stages = stages[1:]
                self.mg_live = None
            for _ in mg:
                pass
        if not self.defer_mod:
            with fw.scope():
                for _ in self.mod_gen(1, 0):
                    pass
        for st in stages:
            if st[0] == "ffn":
                self.ffn(st[1], st[2])
            elif st[0] == "mix":
                self.parts = st[2] if len(st) > 2 else ("ssm", "fox", "ret", "merge")
                self.mixer(st[1])
        self.final()
        self.fw.close()

def prep_shared(inp):
    m = {}
    for l in range(L):
        m[f"ada_w{l}"] = np.ascontiguousarray(inp["ada_w"][l])
        m[f"ada_bT{l}"] = np.ascontiguousarray(inp["ada_b"][l].reshape(9*KC, 128).T)
        m[f"norm_wT{l}"] = np.ascontiguousarray(inp["norm_w"][l].reshape(3*KC, 128).T)
        for i in range(2):
            m[f"w1_{l}_{i}"] = np.ascontiguousarray(inp["ffn_w1"][l, i])
            m[f"w3_{l}_{i}"] = np.ascontiguousarray(inp["ffn_w3"][l, i])
            m[f"w2_{l}_{i}"] = np.ascontiguousarray(inp["ffn_w2"][l, i])
    m["fnorm_wT"] = np.ascontiguousarray(inp["final_norm_w"].reshape(KC, 128).T)
    return m
def prep_core(inp, b):
    return {"xT": np.ascontiguousarray(inp["x"][b].T), "cT": np.ascontiguousarray(inp["c"][b].reshape(KC,128).T)}

def prep_shared_mix(inp, m):
    f32 = np.float32
    for l in range(L):
        m[f"w_in{l}"] = np.ascontiguousarray(inp["w_in"][l])
        m[f"fbias{l}"] = np.ascontiguousarray(inp["fox_f_bias"][l].reshape(8, 1))
        m[f"glu_w{l}"] = np.ascontiguousarray(inp["glu_w"][l])
        m[f"glu_bT{l}"] = np.ascontiguousarray(inp["glu_b"][l].reshape(8, 128).T)
        m[f"gn_wT{l}"] = np.ascontiguousarray(inp["ret_gn_w"][l].reshape(8, 128).T)
        m[f"w_br{l}"] = np.ascontiguousarray(inp["w_branch"][l].reshape(3 * 1024, 2048))
        m[f"b_gateT{l}"] = np.ascontiguousarray(inp["b_gate"][l].reshape(48, 128).T)
        m[f"w_out{l}"] = np.ascontiguousarray(inp["w_out"][l])
        A_re, A_im, ldt = inp["ssm_A_re"][l], inp["ssm_A_im"][l], inp["ssm_log_dt"][l]
        def stl(A):
            return np.ascontiguousarray(A.reshape(32, 2, 64).transpose(1, 2, 0).reshape(128, 32))
        def rowl(A):
            a = A.reshape(8, 8, 1, 64)
            a = np.broadcast_to(a, (8, 8, 16, 64))
            return np.ascontiguousarray(a.transpose(1, 2, 0, 3).reshape(128, 512))
        m[f"sA_reS{l}"] = stl(A_re)
        m[f"sA_imS{l}"] = stl(A_im)
        m[f"sldtS{l}"] = stl(np.broadcast_to(ldt[:, None], (64, 64)))
        m[f"sA_reR{l}"] = rowl(A_re)
        m[f"sA_imR{l}"] = rowl(A_im)
        m[f"sldtR{l}"] = rowl(np.broadcast_to(ldt[:, None], (64, 64)))
        def browl(B):
            b = B.reshape(8, 8, 64, 16)
            return np.ascontiguousarray(b.transpose(1, 3, 0, 2).reshape(128, 512))
        m[f"sB_reR{l}"] = browl(inp["ssm_B_re"][l])
        m[f"sB_imR{l}"] = browl(inp["ssm_B_im"][l])
        def cstl(C):
            c = C.reshape(32, 2, 16, 64)
            return np.ascontiguousarray(c.transpose(1, 3, 0, 2).reshape(128, 512))
        m[f"sC_reS{l}"] = cstl(inp["ssm_C_re"][l])
        m[f"sC_imS{l}"] = cstl(inp["ssm_C_im"][l])
        m[f"sD_T{l}"] = np.ascontiguousarray(inp["ssm_D"][l].reshape(8, 128).T)
    k = np.arange(128)
    m["c_tri"] = (k[:, None] <= k[None, :]).astype(f32)
    pos = np.arange(S, dtype=f32)
    inv_freq = (1.0 / (f32(10000.0) ** np.linspace(0.0, 1.0, 64, dtype=f32))).astype(f32)
    ang = (pos[:, None] * inv_freq[None, :]).astype(f32)
    cos = np.cos(ang).astype(f32).T
    sin = np.sin(ang).astype(f32).T
    m["c_rotC"] = np.ascontiguousarray(np.concatenate([cos, cos], 0))
    m["c_rotS"] = np.ascontiguousarray(np.concatenate([-sin, sin], 0))
    gf = np.zeros((8, 128, 512), f32)
    tt = np.zeros((8, 128, 512), f32)
    mm = np.arange(128)[:, None].astype(np.float64)
    nn = np.arange(512)[None, :].astype(np.float64)
    for h in range(8):
        lg = np.log(np.float64(1.0 - 2.0 ** (-5.0 - h)))
        gf[h] = np.exp((nn - mm + 128) * lg)
        tt[h] = np.where(nn >= mm, np.exp(np.maximum(nn - mm, 0) * lg), 0.0)
    m["c_gfull"] = gf
    m["c_ttri"] = tt
    c = np.arange(128)
    m["c_mrow"] = np.stack([((c // 32 == jj) & ((c // 16) % 2 == g2)) for jj in range(4) for g2 in range(2)], 1).astype(f32)
    m["c_mst"] = np.stack([(c // 64 == g2) for g2 in range(2)], 1).astype(f32)
    return m


_CACHE = {}


def _stages():
    st = []
    for l in range(L):
        st += [("ffn", l, 0), ("mix", l), ("ffn", l, 1)]
    return st


def kernel(**inputs):
    inp = {k: np.asarray(v) for k, v in inputs.items()}
    if "nc" not in _CACHE:
        nc = bass.Bass("TRN2", target_bir_lowering=False)
        kbo = KBM(nc, _stages(), debug=False)
        kbo.build()
        names = set()
        for a in nc.allocations:
            if isinstance(a, mybir.MemoryLocationSet) and a.kind == "ExternalInput":
                names.add(a.memorylocations[0].name)
        _CACHE["nc"] = nc
        _CACHE["names"] = names
    nc, names = _CACHE["nc"], _CACHE["names"]
    shared = prep_shared_mix(inp, prep_shared(inp))
    in_maps = []
    for b in range(8):
        m = {**shared, **prep_core(inp, b)}
        in_maps.append({k: v for k, v in m.items() if k in names})
    res = run_bass_kernel_spmd(nc, in_maps, core_ids=list(range(8)))
    out = np.stack([np.ascontiguousarray(np.asarray(r["outT"]).T) for r in res.results], 0)
    return out.astype(np.float32)
```
